# Optimizing a Trainium2 kernel written in Bass

```python
import math
import jax, jax.numpy as jnp
from jax import lax
import numpy as np

D_MODEL = 2048
BATCH = 4
SEQ = 4096
DEPTH = 1

D_ATTN = D_MODEL // 2
D_LRU = D_MODEL - D_ATTN
HEAD_DIM = 64
N_HEADS = D_ATTN // HEAD_DIM
DILATED_PATTERNS = ((128, 1), (512, 4), (2048, 16))
LRU_BLOCKS = 16
LRU_BLOCK_DIM = D_LRU // LRU_BLOCKS
CONV_WIDTH = 4
RG_C = 8.0
D_PROJ = 3 * D_ATTN + 2 * D_LRU
N_GROUPS = 4
EXPERTS_PER_GROUP = 8
N_EXPERTS = N_GROUPS * EXPERTS_PER_GROUP
TOP_K = 2
D_EXPERT = 512
MOE_BLOCK = 128
ALPHA = (2.0 * DEPTH) ** 0.25
BETA = (8.0 * DEPTH) ** -0.25
LN_EPS = 1e-5
RMS_EPS = 1e-6

kernel_name = "hymba_dilated_rglru_hmoe_deepnorm"


def _layer_norm(x, g, b):
    xf = x.astype(jnp.float32)
    mu = jnp.mean(xf, axis=-1, keepdims=True)
    var = jnp.mean(jnp.square(xf - mu), axis=-1, keepdims=True)
    return ((xf - mu) * lax.rsqrt(var + LN_EPS) * g.astype(jnp.float32) + b.astype(jnp.float32)).astype(x.dtype)


def _rms_norm(x, g):
    xf = x.astype(jnp.float32)
    return xf * lax.rsqrt(jnp.mean(jnp.square(xf), axis=-1, keepdims=True) + RMS_EPS) * g.astype(jnp.float32)


def _dilated_band_attention(q, k, v, slopes, window, dilation):
    B, S, H, Dh = q.shape
    w = window // dilation
    L = S // dilation
    nb = -(-L // w)
    Lp = nb * w

    def to_sub(t):
        t = t.reshape(B, L, dilation, H, Dh).transpose(0, 2, 1, 3, 4).reshape(B * dilation, L, H, Dh)
        t = jnp.pad(t, ((0, 0), (0, Lp - L), (0, 0), (0, 0)))
        return t.reshape(B * dilation, nb, w, H, Dh)

    def with_prev(t):
        prev = jnp.pad(t[:, :-1], ((0, 0), (1, 0), (0, 0), (0, 0), (0, 0)))
        return jnp.concatenate([prev, t], axis=2)

    qb = to_sub(q)
    kk = with_prev(to_sub(k))
    vv = with_prev(to_sub(v))
    s = jnp.einsum('bnqhd,bnkhd->bnhqk', qb, kk)
    qi = jnp.arange(w)[:, None]
    ki = jnp.arange(2 * w)[None, :]
    dist = qi + w - ki
    blk = jnp.arange(nb)[:, None, None]
    valid = (dist >= 0) & (dist <= w) & (blk * w + ki - w >= 0)
    bias = -slopes[:, None, None] * (dist * dilation).astype(jnp.float32)[None]
    s = jnp.where(valid[None, :, None], s + bias[None, None], -jnp.inf)
    m = jnp.max(s, axis=-1, keepdims=True)
    p = jnp.exp(s - m)
    l = jnp.sum(p, axis=-1, keepdims=True)
    o = jnp.einsum('bnhqk,bnkhd->bnqhd', p / l, vv)
    lse = (m + jnp.log(l))[..., 0].transpose(0, 1, 3, 2)
    o = o.reshape(B, dilation, Lp, H, Dh)[:, :, :L].transpose(0, 2, 1, 3, 4).reshape(B, S, H, Dh)
    lse = lse.reshape(B, dilation, Lp, H)[:, :, :L].transpose(0, 2, 1, 3).reshape(B, S, H)
    return o, lse


def _causal_depthwise_conv(x, w, b):
    S = x.shape[1]
    xp = jnp.pad(x, ((0, 0), (CONV_WIDTH - 1, 0), (0, 0)))
    y = b
    for j in range(CONV_WIDTH):
        y = y + xp[:, j:j + S] * w[j]
    return y


def _rg_lru(x, wa, ba, wx, bx, lam):
    B, S, C = x.shape
    xb = x.reshape(B, S, LRU_BLOCKS, LRU_BLOCK_DIM)
    r = jax.nn.sigmoid(jnp.einsum('bsnd,nde->bsne', xb, wa) + ba).reshape(B, S, C)
    i = jax.nn.sigmoid(jnp.einsum('bsnd,nde->bsne', xb, wx) + bx).reshape(B, S, C)
    log_a = -RG_C * r * jax.nn.softplus(-lam)
    a = jnp.exp(log_a)
    u = jnp.sqrt(-jnp.expm1(2.0 * log_a)) * (i * x)

    def combine(c1, c2):
        a1, b1 = c1
        a2, b2 = c2
        return a1 * a2, a2 * b1 + b2

    _, h = lax.associative_scan(combine, (a, u), axis=1)
    return h


def _hybrid_mixer(x, w_in, conv_w, conv_b, lru_wa, lru_ba, lru_wx, lru_bx, lru_lambda,
                  attn_norm_g, lru_norm_g, w_out):
    B, S, _ = x.shape
    f32 = jnp.float32
    proj = jnp.einsum('bsd,de->bse', x, w_in)
    q, k, v, xr, xg = jnp.split(proj, [D_ATTN, 2 * D_ATTN, 3 * D_ATTN, 3 * D_ATTN + D_LRU], axis=-1)
    q = q.reshape(B, S, N_HEADS, HEAD_DIM).astype(f32) * (HEAD_DIM ** -0.5)
    k = k.reshape(B, S, N_HEADS, HEAD_DIM).astype(f32)
    v = v.reshape(B, S, N_HEADS, HEAD_DIM).astype(f32)
    slopes = jnp.exp2(-8.0 * jnp.arange(1, N_HEADS + 1, dtype=f32) / N_HEADS)
    outs, lses = [], []
    for window, dilation in DILATED_PATTERNS:
        o, l = _dilated_band_attention(q, k, v, slopes, window, dilation)
        outs.append(o)
        lses.append(l)
    wts = jax.nn.softmax(jnp.stack(lses), axis=0)
    attn = jnp.sum(wts[..., None] * jnp.stack(outs), axis=0).reshape(B, S, D_ATTN)
    xr = _causal_depthwise_conv(xr.astype(f32), conv_w.astype(f32), conv_b.astype(f32))
    h = _rg_lru(xr, lru_wa.astype(f32), lru_ba.astype(f32), lru_wx.astype(f32), lru_bx.astype(f32),
                lru_lambda.astype(f32))
    rec = h * jax.nn.gelu(xg.astype(f32))
    y = jnp.concatenate([_rms_norm(attn, attn_norm_g), _rms_norm(rec, lru_norm_g)], axis=-1).astype(x.dtype)
    return jnp.einsum('bse,ed->bsd', y, w_out)


def _hier_moe(x2d, grp_w, grp_b, exp_w, exp_b, w1, w3, w2):
    T, D = x2d.shape
    f32 = jnp.float32
    xf = x2d.astype(f32)
    p_grp = jax.nn.softmax(xf @ grp_w.astype(f32) + grp_b.astype(f32), axis=-1)
    g_idx = jnp.argmax(p_grp, axis=-1)
    g_gate = jnp.take_along_axis(p_grp, g_idx[:, None], axis=-1)
    e_logits = (xf @ exp_w.astype(f32) + exp_b.astype(f32)).reshape(T, N_GROUPS, EXPERTS_PER_GROUP)
    e_logits = jnp.take_along_axis(e_logits, g_idx[:, None, None], axis=1)[:, 0]
    top_v, top_i = lax.top_k(jax.nn.softmax(e_logits, axis=-1), TOP_K)
    weights = g_gate * top_v / jnp.sum(top_v, axis=-1, keepdims=True)
    e_flat = (g_idx[:, None] * EXPERTS_PER_GROUP + top_i).reshape(-1).astype(jnp.int32)
    w_flat = weights.reshape(-1).astype(x2d.dtype)
    tok_flat = jnp.repeat(jnp.arange(T, dtype=jnp.int32), TOP_K)
    order = jnp.argsort(e_flat)
    e_sorted, tok_sorted, w_sorted = e_flat[order], tok_flat[order], w_flat[order]
    counts = jnp.zeros((N_EXPERTS,), jnp.int32).at[e_flat].add(1)
    padded = (counts + MOE_BLOCK - 1) // MOE_BLOCK * MOE_BLOCK
    pad_end = jnp.cumsum(padded)
    pad_start = pad_end - padded
    start = jnp.cumsum(counts) - counts
    dest = pad_start[e_sorted] + (jnp.arange(T * TOP_K, dtype=jnp.int32) - start[e_sorted])
    P = ((T * TOP_K + MOE_BLOCK - 1) // MOE_BLOCK + N_EXPERTS) * MOE_BLOCK
    buf_tok = jnp.full((P,), T, jnp.int32).at[dest].set(tok_sorted)
    buf_w = jnp.zeros((P,), x2d.dtype).at[dest].set(w_sorted)
    n_blk = P // MOE_BLOCK
    blk_e = jnp.minimum(jnp.searchsorted(pad_end, jnp.arange(n_blk, dtype=jnp.int32) * MOE_BLOCK, side='right'),
                        N_EXPERTS - 1)
    x_pad = jnp.concatenate([x2d, jnp.zeros((1, D), x2d.dtype)], axis=0)
    xb = x_pad[buf_tok].reshape(n_blk, MOE_BLOCK, D)

    def expert_block(args):
        xblk, e = args
        return (jax.nn.silu(xblk @ w1[e]) * (xblk @ w3[e])) @ w2[e]

    yb = lax.map(expert_block, (xb, blk_e)).reshape(P, D)
    out = jnp.zeros((T + 1, D), x2d.dtype).at[buf_tok].add(yb * buf_w[:, None])
    return out[:T]


def setup_inputs(seed: int = 0) -> dict:
    key = jax.random.key(seed)
    ks = jax.random.split(key, 24)
    f32 = jnp.float32
    nrm = lambda k, shape, s: jax.random.normal(k, shape, f32) * s
    col_scale = jnp.concatenate([jnp.ones((2 * D_ATTN,), f32), jnp.full((D_ATTN,), BETA, f32),
                                 jnp.ones((2 * D_LRU,), f32)])
    a0 = jax.random.uniform(ks[8], (DEPTH, D_LRU), f32, 0.9, 0.999)
    sa = a0 ** (1.0 / RG_C)
    return {
        "x": nrm(ks[0], (BATCH, SEQ, D_MODEL), 1.0),
        "w_in": nrm(ks[1], (DEPTH, D_MODEL, D_PROJ), D_MODEL ** -0.5) * col_scale,
        "conv_w": nrm(ks[2], (DEPTH, CONV_WIDTH, D_LRU), CONV_WIDTH ** -0.5),
        "conv_b": nrm(ks[3], (DEPTH, D_LRU), 0.02),
        "lru_wa": nrm(ks[4], (DEPTH, LRU_BLOCKS, LRU_BLOCK_DIM, LRU_BLOCK_DIM), LRU_BLOCK_DIM ** -0.5),
        "lru_ba": nrm(ks[5], (DEPTH, LRU_BLOCKS, LRU_BLOCK_DIM), 0.02),
        "lru_wx": nrm(ks[6], (DEPTH, LRU_BLOCKS, LRU_BLOCK_DIM, LRU_BLOCK_DIM), LRU_BLOCK_DIM ** -0.5),
        "lru_bx": nrm(ks[7], (DEPTH, LRU_BLOCKS, LRU_BLOCK_DIM), 0.02),
        "lru_lambda": jnp.log(sa) - jnp.log1p(-sa),
        "attn_norm_g": 1.0 + nrm(ks[9], (DEPTH, D_ATTN), 0.02),
        "lru_norm_g": 1.0 + nrm(ks[10], (DEPTH, D_LRU), 0.02),
        "w_out": nrm(ks[11], (DEPTH, D_MODEL, D_MODEL), BETA * D_MODEL ** -0.5),
        "ln1_g": 1.0 + nrm(ks[12], (DEPTH, D_MODEL), 0.02),
        "ln1_b": nrm(ks[13], (DEPTH, D_MODEL), 0.02),
        "router_grp_w": nrm(ks[14], (DEPTH, D_MODEL, N_GROUPS), D_MODEL ** -0.5),
        "router_grp_b": nrm(ks[15], (DEPTH, N_GROUPS), 0.01),
        "router_exp_w": nrm(ks[16], (DEPTH, D_MODEL, N_EXPERTS), D_MODEL ** -0.5),
        "router_exp_b": nrm(ks[17], (DEPTH, N_EXPERTS), 0.01),
        "w1": nrm(ks[18], (DEPTH, N_EXPERTS, D_MODEL, D_EXPERT), D_MODEL ** -0.5),
        "w3": nrm(ks[19], (DEPTH, N_EXPERTS, D_MODEL, D_EXPERT), D_MODEL ** -0.5),
        "w2": nrm(ks[20], (DEPTH, N_EXPERTS, D_EXPERT, D_MODEL), BETA * D_EXPERT ** -0.5),
        "ln2_g": 1.0 + nrm(ks[21], (DEPTH, D_MODEL), 0.02),
        "ln2_b": nrm(ks[22], (DEPTH, D_MODEL), 0.02),
    }


def reference(x, w_in, conv_w, conv_b, lru_wa, lru_ba, lru_wx, lru_bx, lru_lambda, attn_norm_g, lru_norm_g,
              w_out, ln1_g, ln1_b, router_grp_w, router_grp_b, router_exp_w, router_exp_b, w1, w3, w2,
              ln2_g, ln2_b):
    B, S, D = x.shape
    for layer in range(DEPTH):
        mix = _hybrid_mixer(x, w_in[layer], conv_w[layer], conv_b[layer], lru_wa[layer], lru_ba[layer],
                            lru_wx[layer], lru_bx[layer], lru_lambda[layer], attn_norm_g[layer],
                            lru_norm_g[layer], w_out[layer])
        x = _layer_norm(ALPHA * x + mix, ln1_g[layer], ln1_b[layer])
        moe = _hier_moe(x.reshape(B * S, D), router_grp_w[layer], router_grp_b[layer], router_exp_w[layer],
                        router_exp_b[layer], w1[layer], w3[layer], w2[layer]).reshape(B, S, D)
        x = _layer_norm(ALPHA * x + moe, ln2_g[layer], ln2_b[layer])
    return x
```

```python
import contextlib
import numpy as np
import ml_dtypes
import concourse.bass as bass
import concourse.mybir as mybir
from concourse.alu_op_type import AluOpType as ALU
from concourse.bass_utils import run_bass_kernel_spmd

F32 = mybir.dt.float32
BF16 = mybir.dt.bfloat16
I32 = mybir.dt.int32
AF = mybir.ActivationFunctionType
AX = mybir.AxisListType

D = 2048
SEQ = 4096
NB = 4
HALF = 2048
NH = 16
DLRU = 1024
DPROJ = 5120
NE = 32
DE = 512
CAP = 256
ALPHA = 2.0 ** 0.25
LN_EPS = 1e-5
RMS_EPS = 1e-6
PATTERNS = (1, 4, 16)
WIN = 256
LW = 128
AW = 128
BIG = 1.0e6
NSTG = 8

ENGS = ("sync", "scalar", "vector", "gpsimd", "tensor")


class Sched:
    def __init__(self, nc, self_sync=True):
        self.nc = nc
        self.self_sync = self_sync
        self.q = {e: [] for e in ENGS}
        self.cnt = {}
        self.waited = {e: {} for e in ENGS}
        self.last_w = {}
        self.readers = {}
        self.sems = {}
        self.semkeys = []
        self.final = []
        self.bar = {}

    def barrier(self):
        self.bar = dict(self.cnt)

    def _semkey(self, k):
        if k not in self.cnt:
            self.cnt[k] = 0
            self.semkeys.append(k)
        return k

    def _deps(self, eng, r, w):
        deps = {}

        def add(t):
            if t is None:
                return
            k, v = t
            if deps.get(k, 0) < v:
                deps[k] = v
        for key in r:
            add(self.last_w.get(key))
        for key in w:
            add(self.last_w.get(key))
            for t in self.readers.get(key, ()):
                add(t)
        for k, v in self.bar.items():
            if v:
                add((k, v))
        out = []
        for k, v in deps.items():
            if k == eng and (eng != "vector" or not self.self_sync):
                continue
            if self.waited[eng].get(k, 0) >= v:
                continue
            self.waited[eng][k] = v
            out.append((k, v))
        return out

    def _record(self, ticket, r, w):
        for key in w:
            self.last_w[key] = ticket
            self.readers[key] = []
        for key in r:
            self.readers.setdefault(key, []).append(ticket)

    def op(self, eng, fn, r=(), w=()):
        waits = self._deps(eng, r, w)
        k = self._semkey(eng)
        self.cnt[k] += 1
        ticket = (k, self.cnt[k])
        self.q[eng].append((fn, waits, k, 1))
        self._record(ticket, r, w)
        return ticket

    def dma(self, eng, fn, sem, r=(), w=(), final=False):
        waits = self._deps(eng, r, w)
        k = self._semkey("dma:" + sem)
        self.cnt[k] += 16
        ticket = (k, self.cnt[k])
        self.q[eng].append((fn, waits, k, 16))
        self._record(ticket, r, w)
        if final:
            self.final.append(ticket)
        return ticket

    def emit(self):
        nc = self.nc
        with contextlib.ExitStack() as st:
            for k in self.semkeys:
                self.sems[k] = st.enter_context(nc.semaphore(k.replace(":", "_")))
            fin = {}
            for k, v in self.final:
                fin[k] = max(fin.get(k, 0), v)
            block = st.enter_context(nc.Block())
            for e in ENGS:
                ops = self.q[e]
                extra = fin if e == "sync" else {}
                if not ops and not extra:
                    continue

                def body(eh, ops=ops, extra=extra):
                    for fn, waits, k, inc in ops:
                        for wk, wv in waits:
                            eh.wait_ge(self.sems[wk], wv)
                        fn(eh).then_inc(self.sems[k], inc)
                    for wk, wv in extra.items():
                        eh.wait_ge(self.sems[wk], wv)

                getattr(block, e)(body)


def build_program(stop_after=None, debug=False, skip_lru=False):
    nc = bass.Bass("TRN2", target_bir_lowering=False)

    def din(name, shape, dt=F32):
        return nc.dram_tensor(name, list(shape), dt, kind="ExternalInput").ap()

    xT = din("xT", [2 * HALF // LW, 128, 16 * LW])
    xtok = din("xtok", [HALF, D])
    w_in = din("w_in", [D, DPROJ])
    w_out = din("w_out", [D, D])
    convw = din("convw", [128, 8, 4])
    convb = din("convb", [128, 8])
    ba_d = din("lba", [128, 8])
    bx_d = din("lbx", [128, 8])
    lam_d = din("lam", [128, 8])
    wbda_d = din("wbda", [128, 8, 128])
    wbdx_d = din("wbdx", [128, 8, 128])
    gcat_d = din("gcat", [128, 16])
    ln1g_d = din("ln1g", [128, D])
    ln1b_d = din("ln1b", [128, D])
    ln2g_d = din("ln2g", [128, D])
    ln2b_d = din("ln2b", [128, D])
    wr_d = din("wr", [D, 36])
    br_d = din("br", [128, 36])
    ne_in = NE if stop_after is None else 1
    w1_d = din("w1", [ne_in, 128, 16 * DE])
    w3_d = din("w3", [ne_in, 128, 16 * DE])
    w2_d = din("w2", [ne_in, DE, D])
    etab_d = din("etab", [128, 3, NH, 256])
    flag_d = din("flag", [128, 1])
    ident_d = din("ident", [128, 128])
    tri_d = din("tri", [128, 128])
    ebase_d = din("ebase", [128, NE])
    out_d = nc.dram_tensor("out", [HALF, D], F32, kind="ExternalOutput").ap()
    dbg = {}
    if debug:
        dbg["yT"] = nc.dram_tensor("dbg_yT", [128, 16, HALF], F32, kind="ExternalOutput").ap()
        dbg["ssq"] = nc.dram_tensor("dbg_ssq", [128, 32], F32, kind="ExternalOutput").ap()
        dbg["x1"] = nc.dram_tensor("dbg_x1", [HALF, D], F32, kind="ExternalOutput").ap()
        dbg["rt"] = nc.dram_tensor("dbg_rt", [128, 16, 4], F32, kind="ExternalOutput").ap()
    x1s = nc.dram_tensor("x1s", [HALF, D], F32).ap()
    xs_d = nc.dram_tensor("xs", [NE * CAP, D], BF16).ap()
    ys_d = nc.dram_tensor("ys", [NE * CAP, D], F32).ap()

    S = Sched(nc)
    dmaq = ["sync"]
    dctr = [0]

    def hwq():
        dctr[0] += 1
        return dmaq[dctr[0] % len(dmaq)]

    def MM(out, lhsT, rhs, start, stop, r, w):
        S.op("tensor", lambda e: e.matmul(out, lhsT, rhs, start=start, stop=stop), r, w)

    def MMG(out, pairs, r, w):
        n = len(pairs)
        for i, (lhsT, rhs) in enumerate(pairs):
            edge = i == 0 or i == n - 1
            MM(out, lhsT, rhs, i == 0, i == n - 1, r if edge else [], w if edge else [])

    def TR(out, in_, ident, r, w):
        S.op("tensor", lambda e: e.transpose(out, in_, ident), r, w)

    def ACT(out, in_, func, r, w, bias=None, scale=None, accum=None):
        kw = {}
        if bias is not None:
            kw["bias"] = bias
        if scale is not None:
            kw["scale"] = scale
        if accum is not None:
            kw["accum_out"] = accum
        S.op("scalar", lambda e: e.activation(out=out, in_=in_, func=func, **kw), r, w)

    def TS(eng, out, in0, s1, s2, op0, op1, r, w):
        if op1 is None:
            S.op(eng, lambda e: e.tensor_scalar(out, in0, s1, None, op0), r, w)
        else:
            S.op(eng, lambda e: e.tensor_scalar(out, in0, s1, s2, op0, op1), r, w)

    def STT(out, in0, scalar, in1, op0, op1, r, w):
        S.op("vector", lambda e: e.scalar_tensor_tensor(out, in0, scalar, in1, op0, op1), r, w)

    def TT(eng, out, in0, in1, op, r, w):
        S.op(eng, lambda e: e.tensor_tensor(out, in0, in1, op), r, w)

    def CP(eng, out, in_, r, w):
        S.op(eng, lambda e: e.tensor_copy(out=out, in_=in_), r, w)

    def MS(eng, ap, val, w):
        S.op(eng, lambda e: e.memset(ap, val), (), w)

    def REC(out, in_, r, w):
        S.op("vector", lambda e: e.reciprocal(out=out, in_=in_), r, w)

    def RED(out, in_, op, r, w):
        S.op("vector", lambda e: e.tensor_reduce(out=out, in_=in_, axis=AX.X, op=op), r, w)

    def BNS(out, in_, r, w):
        S.op("vector", lambda e: e.bn_stats(out=out, in_=in_), r, w)

    def BNA(out, in_, r, w):
        S.op("vector", lambda e: e.bn_aggr(out=out, in_=in_), r, w)

    def SCAN(out, d0, d1, init, r, w):
        S.op("vector", lambda e: e.tensor_tensor_scan(out=out, data0=d0, data1=d1, initial=init, op0=ALU.mult, op1=ALU.add), r, w)

    bcreg = {}

    def bc(e):
        if "r" not in bcreg:
            bcreg["r"] = e.to_reg(NE * CAP - 1)
        return bcreg["r"]

    def SCAT(dram, idx, src, sem, r, w):
        S.dma("gpsimd", lambda e: e.indirect_dma_start(out=dram, out_offset=bass.IndirectOffsetOnAxis(ap=idx, axis=0), in_=src, in_offset=None,
                                                       bounds_check=bc(e), oob_is_err=False), sem, r, w)

    def GATH(dst, dram, idx, sem, r, w):
        S.dma("gpsimd", lambda e: e.indirect_dma_start(out=dst, out_offset=None, in_=dram, in_offset=bass.IndirectOffsetOnAxis(ap=idx, axis=0),
                                                       bounds_check=bc(e), oob_is_err=False), sem, r, w)

    def DMA(eng, out, in_, sem, r, w, final=False):
        S.dma(eng, lambda e: e.dma_start(out=out, in_=in_), sem, r, w, final=final)

    with contextlib.ExitStack() as top:
        uniq = [0]

        def sb(st, name, shape, dt=F32):
            uniq[0] += 1
            return st.enter_context(nc.sbuf_tensor(f"s{uniq[0]}_{name}", list(shape), dt))

        def ps(st, name, shape, dt=F32):
            uniq[0] += 1
            return st.enter_context(nc.psum_tensor(f"p{uniq[0]}_{name}", list(shape), dt))

        ident_f = sb(top, "ident_f", [128, 128])
        ident_b = sb(top, "ident_b", [128, 128], BF16)
        flag = sb(top, "flag", [128, 1])
        ones_b = sb(top, "ones_b", [128, 128], BF16)
        pones_b = sb(top, "pones_b", [128, 128], BF16)
        ones_f = sb(top, "ones_f", [128, 128])
        onescol_b = sb(top, "onescol_b", [128, 1], BF16)
        ssq = sb(top, "ssq", [128, 32])
        slots_i = sb(top, "slots_i", [128, 16, 2], I32)
        wts = sb(top, "wts", [128, 16, 2])
        DMA("sync", ident_f[:], ident_d, "k1", (), ["ident_f"])
        DMA("sync", flag[:], flag_d, "k2", (), ["flag"])
        CP("vector", ident_b[:], ident_f[:], ["ident_f"], ["ident_b"])
        MS("vector", ones_b[:], 1.0, ["ones_b"])
        MS("vector", ones_f[:], 1.0, ["ones_f"])
        MS("vector", onescol_b[:], 1.0, ["onescol_b"])
        TS("vector", pones_b[:], ones_f[:], flag[:, 0:1], None, ALU.mult, None, ["ones_f", "flag"], ["pones_b"])
        MS("vector", ssq[:], 0.0, ["ssq"])

        with contextlib.ExitStack() as mix:
            mix.callback(S.barrier)
            yT = sb(mix, "yT", [128, 16, HALF], BF16)

            with contextlib.ExitStack() as ph:
                ph.callback(S.barrier)
                wl = sb(ph, "wl", [128, 16, 2048], BF16)
                cw = sb(ph, "cw", [128, 8, 4])
                cb = sb(ph, "cb", [128, 8])
                lba = sb(ph, "lba", [128, 8])
                lbx = sb(ph, "lbx", [128, 8])
                cl = sb(ph, "cl", [128, 8])
                cl2 = sb(ph, "cl2", [128, 8])
                wbda = sb(ph, "wbda", [128, 8, 128], BF16)
                wbdx = sb(ph, "wbdx", [128, 8, 128], BF16)
                stg = [sb(ph, "lstg0", [128, 2048])] * 2
                xw = [sb(ph, f"lxw{i}", [128, 16, LW], BF16) for i in range(2)]
                xr = sb(ph, "xr", [128, 8, LW + 3])
                xc2 = [sb(ph, f"xc{i}", [128, 8, LW]) for i in range(2)]
                xcb2 = [sb(ph, f"xcb{i}", [128, 8, LW], BF16) for i in range(2)]
                gr2 = [sb(ph, "gr0", [128, 8, LW])] * 2
                gi2 = [sb(ph, f"gi{i}", [128, 8, LW]) for i in range(2)]
                ga2 = [sb(ph, f"ga{i}", [128, 8, LW]) for i in range(2)]
                gs2 = [sb(ph, "gs0", [128, 8, LW])] * 2
                hh = [sb(ph, f"hh{i}", [128, 8, LW]) for i in range(2)]
                gg2 = [sb(ph, f"gg{i}", [128, 8, LW]) for i in range(2)]
                pxr = [ps(ph, f"pxr{i}", [128, 2, LW]) for i in range(4)]
                pg = [ps(ph, f"pg{i}", [128, 2, LW]) for i in range(2)]
                pss = ps(ph, "pss", [128, 16])

                for t, d_, kn in ((cw, convw, "cw"), (cb, convb, "cb"), (lba, ba_d, "lba"), (lbx, bx_d, "lbx"), (cl, lam_d, "cl")):
                    DMA("sync", t[:], d_, "c2" + kn, (), [kn])
                ACT(cl[:], cl[:], AF.Exp, ["cl"], ["cl"], scale=-1.0)
                TS("vector", cl2[:], cl[:], -0.25, 1.0 / 3.0, ALU.mult, ALU.add, ["cl"], ["cl2"])
                TT("vector", cl2[:], cl2[:], cl[:], ALU.mult, ["cl", "cl2"], ["cl2"])
                TS("vector", cl2[:], cl2[:], -0.5, None, ALU.add, None, ["cl2"], ["cl2"])
                TT("vector", cl2[:], cl2[:], cl[:], ALU.mult, ["cl", "cl2"], ["cl2"])
                TS("vector", cl2[:], cl2[:], 1.0, None, ALU.add, None, ["cl2"], ["cl2"])
                TT("vector", cl[:], cl2[:], cl[:], ALU.mult, ["cl", "cl2"], ["cl"])
                TS("vector", cl2[:], cl[:], -16.0, None, ALU.mult, None, ["cl"], ["cl2"])
                TS("vector", cl[:], cl[:], -8.0, None, ALU.mult, None, ["cl", "cl2"], ["cl"])
                for t, d_, kn in ((wbda, wbda_d, "wbda"), (wbdx, wbdx_d, "wbdx")):
                    DMA("sync", stg[0][:, 0:1024], d_.rearrange("p c m -> p (c m)"), "c3", [], ["lstg0"])
                    CP("vector", t[:].rearrange("p c m -> p (c m)"), stg[0][:, 0:1024], ["lstg0"], [kn])
                w_in_v = w_in.rearrange("(k p) n -> p k n", p=128)
                for k in range(16):
                    s_ = k % 2
                    DMA(hwq(), stg[s_][:, 0:2048], w_in_v[:, k, 3072:5120], "lw0", [], ["lstg0"])
                    if k % 2:
                        ACT(wl[:, k, :], stg[s_][:, 0:2048], AF.Copy, ["lstg0"], [("wl", k)])
                    else:
                        CP("vector", wl[:, k, :], stg[s_][:, 0:2048], ["lstg0"], [("wl", k)])
                MS("vector", xr[:], 0.0, [("xr", p_) for p_ in range(4)])
                MS("vector", hh[1][:], 0.0, [("hh1", p_) for p_ in range(4)])
                wl_keys = [("wl", k) for k in range(16)]
                def load_xwin(wi_):
                    DMA(hwq(), stg[wi_ % 2][:], xT[wi_], "lw0", [], ["lstg0"])
                    ACT(xw[wi_ % 2][:].rearrange("p k t -> p (k t)"), stg[wi_ % 2][:], AF.Copy, ["lstg0"], [f"lxw{wi_ % 2}"])

                NWL = 0 if skip_lru else 2 * HALF // LW
                if NWL:
                    load_xwin(0)
                def lru_window(wi):
                    own = wi >= HALF // LW
                    s_ = wi % 2
                    t0 = wi * LW
                    o0 = t0 - HALF
                    wp = wi % 2
                    xc_w, xcb_w, gr_w, gi_w, ga_w, gs_w, gg_w = xc2[wp], xcb2[wp], gr2[wp], gi2[wp], ga2[wp], gs2[wp], gg2[wp]
                    sq_w = xcb_w
                    hcur, hprev = hh[wi % 2], hh[(wi + 1) % 2]
                    hk, hpk = f"hh{wi % 2}", f"hh{(wi + 1) % 2}"
                    for p in range(4):
                        CP("vector", xr[:, 2 * p:2 * p + 2, 0:3], xr[:, 2 * p:2 * p + 2, LW:LW + 3], [("xr", p)], [("xr", p)])
                        for c in (2 * p, 2 * p + 1):
                            MMG(pxr[p][:, c % 2, :], [(wl[:, k, c * 128:(c + 1) * 128], xw[s_][:, k, :]) for k in range(16)],
                                wl_keys + [f"lxw{s_}"], [("pxr", p)])
                        CP("vector", xr[:, 2 * p:2 * p + 2, 3:3 + LW], pxr[p][:, :, :], [("pxr", p)], [("xr", p)])
                    yield
                    if own:
                        for p in range(4):
                            for c in (2 * p, 2 * p + 1):
                                MMG(pxr[p][:, c % 2, :], [(wl[:, k, 1024 + c * 128:1024 + (c + 1) * 128], xw[s_][:, k, :]) for k in range(16)],
                                    wl_keys + [f"lxw{s_}"], [("pxr", p)])
                            ACT(gg_w[:, 2 * p:2 * p + 2, :], pxr[p][:, :, :], AF.Gelu_apprx_tanh, [("pxr", p)], [("gg", wp, p)])
                    yield
                    if wi + 1 < NWL:
                        load_xwin(wi + 1)
                    for p in range(4):
                        for c in (2 * p, 2 * p + 1):
                            TS("vector", xc_w[:, c, :], xr[:, c, 3:3 + LW], cw[:, c, 3:4], cb[:, c:c + 1], ALU.mult, ALU.add,
                               [("xr", p), "cw", "cb"], [("xcc", wp, c)])
                        for j in range(3):
                            for c in (2 * p, 2 * p + 1):
                                STT(xc_w[:, c, :], xr[:, c, j:j + LW], cw[:, c, j:j + 1], xc_w[:, c, :], ALU.mult, ALU.add, [("xr", p), ("xcc", wp, c), "cw"], [("xcc", wp, c)])
                        ACT(xcb_w[:, 2 * p:2 * p + 2, :], xc_w[:, 2 * p:2 * p + 2, :], AF.Copy, [("xcc", wp, 2 * p), ("xcc", wp, 2 * p + 1)], [("xcb", wp, p)])
                    yield
                    for p in range(4):
                        for c in (2 * p, 2 * p + 1):
                            pa = pg[c % 2]
                            MM(pa[:, 0, :], wbda[:, c, :], xcb_w[:, c, :], True, True, ["wbda", ("xcb", wp, p)], [("pg", c % 2)])
                            MM(pa[:, 1, :], wbdx[:, c, :], xcb_w[:, c, :], True, True, ["wbdx", ("xcb", wp, p)], [("pg", c % 2)])
                            ACT(gr_w[:, c, :], pa[:, 0, :], AF.Sigmoid, [("pg", c % 2), "lba"], [("gr", 0, p)], bias=lba[:, c:c + 1])
                            ACT(gi_w[:, c, :], pa[:, 1, :], AF.Sigmoid, [("pg", c % 2), "lbx"], [("gi", wp, p)], bias=lbx[:, c:c + 1])
                    yield
                    for p in range(4):
                        for c in (2 * p, 2 * p + 1):
                            ACT(ga_w[:, c, :], gr_w[:, c, :], AF.Exp, [("gr", 0, p), "cl"], [("ga", wp, p)], scale=cl[:, c:c + 1])
                            ACT(gs_w[:, c, :], gr_w[:, c, :], AF.Exp, [("gr", 0, p), "cl2"], [("gs", 0, p)], scale=cl2[:, c:c + 1])
                    yield
                    for p in range(4):
                        sl2 = slice(2 * p, 2 * p + 2)
                        ACT(gs_w[:, sl2, :], gs_w[:, sl2, :], AF.Sqrt, [("gs", 0, p)], [("gs", 0, p)], bias=1.0, scale=-1.0)
                    yield
                    for p in range(4):
                        sl2 = slice(2 * p, 2 * p + 2)
                        TT("vector", gi_w[:, sl2, :], gi_w[:, sl2, :], xc_w[:, sl2, :], ALU.mult, [("gi", wp, p), ("xcc", wp, 2 * p), ("xcc", wp, 2 * p + 1)], [("gi", wp, p)])
                        if own:
                            TT("vector", gs_w[:, sl2, :], gs_w[:, sl2, :], gi_w[:, sl2, :], ALU.mult, [("gs", 0, p), ("gi", wp, p)], [("gs", 0, p)])
                        else:
                            STT(gs_w[:, sl2, :], gi_w[:, sl2, :], flag[:, 0:1], gs_w[:, sl2, :], ALU.mult, ALU.mult, [("gs", 0, p), ("gi", wp, p), "flag"], [("gs", 0, p)])
                        for c in (2 * p, 2 * p + 1):
                            SCAN(hcur[:, c, :], ga_w[:, c, :], gs_w[:, c, :], hprev[:, c, LW - 1:LW], [("ga", wp, p), ("gs", 0, p), (hpk, p)], [(hk, p)])
                        if own:
                            TT("vector", gg_w[:, sl2, :], gg_w[:, sl2, :], hcur[:, sl2, :], ALU.mult, [("gg", wp, p), (hk, p)], [("gg", wp, p)])
                            ACT(yT[:, 8 + 2 * p:10 + 2 * p, o0:o0 + LW], gg_w[:, sl2, :], AF.Copy, [("gg", wp, p)], [("yT", "l", wi)])
                            TT("vector", sq_w[:, sl2, :], gg_w[:, sl2, :], gg_w[:, sl2, :], ALU.mult, [("gg", wp, p)], [("xcb", wp, p)])
                    if own:
                        for tb in range(LW // 128):
                            tile_i = (o0 + tb * 128) // 128
                            MMG(pss[:, tile_i:tile_i + 1], [(sq_w[:, c, tb * 128:(tb + 1) * 128], onescol_b[:, 0:1]) for c in range(8)],
                                [("xcb", wp, p_) for p_ in range(4)] + ["onescol_b"], ["pss"])

                gens = [lru_window(w_) for w_ in range(NWL)]
                if NWL:
                    for _ in range(3):
                        next(gens[0], None)
                for w_ in range(NWL):
                    nxt = gens[w_ + 1] if w_ + 1 < NWL else None
                    for step in range(4):
                        next(gens[w_], None)
                        if nxt is not None and step < 3:
                            next(nxt, None)
                    for _ in gens[w_]:
                        pass
                if not skip_lru:
                    CP("vector", ssq[:, 16:32], pss[:, 0:16], ["pss"], ["ssq"])
                else:
                    for w_ in range(HALF // LW, 2 * HALF // LW):
                        MS("gpsimd", yT[:, 8:16, (w_ - HALF // LW) * LW:(w_ - HALF // LW + 1) * LW], 0.0, [("yT", "l", w_)])

            if stop_after == "lru":
                pass
            else:
                with contextlib.ExitStack() as ph:
                    ph.callback(S.barrier)
                    accO = sb(ph, "accO", [128, 2, HALF])
                    accD = sb(ph, "accD", [128, 2, HALF])
                    etb = sb(ph, "etb", [128, 3, 4, 256], BF16)
                    qTm = sb(ph, "qTm", [128, 4, HALF], BF16)
                    kT = sb(ph, "kT", [128, 2, 2 * HALF], BF16)
                    vT = sb(ph, "vT", [128, 2, 2 * HALF], BF16)
                    ALV = {"a1": 1, "a2": 2, "a2p0": 2, "a2p1": 2, "a2p2": 2, "a3": 3, "a3p0": 3}.get(stop_after, 9)
                    VPAT = {"a2p0": (0,), "a2p1": (1,), "a2p2": (2,)}.get(stop_after, (0, 1, 2))
                    APAT = PATTERNS[:1] if stop_after == "a3p0" else PATTERNS
                    for pas in range(4 if ALV == 9 else 1):
                        MS("vector", qTm[:], 0.0, ["qTm"])
                        with contextlib.ExitStack() as ip:
                            ip.callback(S.barrier)
                            wq = sb(ip, "wq", [128, 16, 768], BF16)
                            stg = [sb(ip, f"astg{i}", [128, 16 * AW]) for i in range(2)]
                            xw = [sb(ip, f"axw{i}", [128, 16, AW], BF16) for i in range(2)]
                            pq = [ps(ip, f"pq{i}", [128, 2, AW]) for i in range(3)]
                            for pi_ in range(3):
                                s_ = pi_ % 2
                                DMA(hwq(), stg[s_][:, 0:1024].rearrange("p (h q) -> p h q", h=4), etab_d[:, pi_, pas * 4:(pas + 1) * 4, :],
                                    f"aw{s_}", [], [f"astg{s_}"])
                                ACT(etb[:, pi_, :, :], stg[s_][:, 0:1024].rearrange("p (h q) -> p h q", h=4), AF.Copy, [f"astg{s_}"], ["etb"])
                            w_in_v = w_in.rearrange("(k p) n -> p k n", p=128)
                            for k in range(16):
                                s_ = k % 2
                                for j in range(3):
                                    c0 = j * 1024 + pas * 256
                                    DMA(hwq(), stg[s_][:, j * 256:(j + 1) * 256], w_in_v[:, k, c0:c0 + 256], f"aw{s_}", [], [f"astg{s_}"])
                                if k % 2:
                                    ACT(wq[:, k, :], stg[s_][:, 0:768], AF.Copy, [f"astg{s_}"], [("wq", k)])
                                else:
                                    CP("vector", wq[:, k, :], stg[s_][:, 0:768], [f"astg{s_}"], [("wq", k)])
                            def load_xwin_a(wi_):
                                q_ = wi_ % 2
                                hf = 8 * AW
                                DMA(hwq(), stg[q_][:], xT[wi_], f"aw{q_}", [], [f"astg{q_}"])
                                ACT(xw[q_][:].rearrange("p k t -> p (k t)")[:, 0:hf], stg[q_][:, 0:hf], AF.Copy, [f"astg{q_}"], [(f"axw{q_}", 0)])
                                ACT(xw[q_][:].rearrange("p k t -> p (k t)")[:, hf:2 * hf], stg[q_][:, hf:2 * hf], AF.Copy, [f"astg{q_}"], [(f"axw{q_}", 1)])

                            load_xwin_a(0)
                            for wi in range(2 * HALF // AW):
                                own = wi >= HALF // AW
                                s_ = wi % 2
                                t0 = wi * AW
                                if wi + 1 < 2 * HALF // AW:
                                    load_xwin_a(wi + 1)
                                for j in ((1, 2, 0) if own else (1, 2)):
                                    pt = pq[j]
                                    for cc in range(2):
                                        col = j * 256 + cc * 128
                                        MMG(pt[:, cc, :], [(wq[:, k, col:col + 128], xw[s_][:, k, :]) for k in range(16)],
                                            [("wq", k) for k in range(16)] + [(f"axw{s_}", 0), (f"axw{s_}", 1)], [("pq", j)])
                                    if j == 1:
                                        CP("vector", kT[:, :, t0:t0 + AW], pt[:, :, :], [("pq", j)], ["kT"])
                                    elif j == 2:
                                        ACT(vT[:, :, t0:t0 + AW], pt[:, :, :], AF.Copy, [("pq", j)], ["vT"])
                                    else:
                                        o0 = t0 - HALF
                                        for hl in range(4):
                                            po = (hl % 2) * 64
                                            ACT(qTm[po:po + 64, hl, o0:o0 + AW], pt[po:po + 64, hl // 2, :], AF.Copy,
                                                [("pq", j)], ["qTm"], scale=0.125)
                        with contextlib.ExitStack() as at:
                            at.callback(S.barrier)
                            NVB = 69
                            vtok = sb(at, "vtok", [128, NVB, 256], BF16)
                            PT = [sb(at, f"PT{i}", [128, 256], BF16) for i in range(6)]
                            EX = [sb(at, f"EX{i}", [128, 256], BF16) for i in range(6)]
                            sqa = sb(at, "sqa", [128, 2, HALF], BF16)
                            vt = contextlib.ExitStack()
                            ptr = [ps(vt, f"ptr{i}", [128, 256], BF16) for i in range(2)]
                            vmap = {}
                            nv = 0
                            for pi, d in enumerate(PATTERNS if ALV >= 2 else ()):
                                if pi not in VPAT:
                                    continue
                                nblk = 32 // d
                                for r_ in range(d):
                                    for b in range(nblk // 2 - 1, nblk):
                                        vmap[(pi, r_, b)] = nv
                                        st_ = d * 128 * b + r_
                                        pt = ptr[nv % 2]
                                        for cc in range(2):
                                            TR(pt[:, cc * 128:(cc + 1) * 128], vT[:, cc, st_:st_ + d * 127 + 1:d], ident_b[:],
                                               ["vT", "ident_b"], [("ptr", nv % 2)])
                                        if nv % 2:
                                            ACT(vtok[:, nv, :], pt[:], AF.Copy, [("ptr", nv % 2)], [("vtok", nv)])
                                        else:
                                            CP("vector", vtok[:, nv, :], pt[:], [("ptr", nv % 2)], [("vtok", nv)])
                                        nv += 1
                            assert nv == NVB or ALV < 9
                            S.barrier()
                            vt.close()
                            pST = [ps(at, f"pST{i}", [128, 256]) for i in range(4)]
                            pO = [ps(at, f"pO{i}", [128, 512]) for i in range(2)]
                            pD = [ps(at, f"pD{i}", [128, 512]) for i in range(2)]
                            items = []
                            gh_ = 0
                            for pi, d in enumerate(APAT if ALV >= 3 else ()):
                                nblk = 32 // d
                                groups = []
                                if d == 1:
                                    for g in range(4):
                                        groups.append(([(0, 16 + 4 * g + u) for u in range(4)],
                                                       lambda a, g=g: a[:, 512 * g:512 * (g + 1)]))
                                elif d == 4:
                                    for c_ in range(4):
                                        groups.append(([(c_, 4 + u) for u in range(4)],
                                                       lambda a, c_=c_: a[:, c_:HALF:4]))
                                else:
                                    for g in range(4):
                                        groups.append(([(4 * g + u, 1) for u in range(4)],
                                                       lambda a, g=g: a.rearrange("p (a r) -> p r a", r=16)[:, 4 * g:4 * g + 4, :]))
                                for units, accview in groups:
                                    for hl in range(4):
                                        for u, (r_, n) in enumerate(units):
                                            items.append((pi, d, nblk, hl, gh_, u, r_, n, accview, u == 3))
                                        gh_ += 1
                            NST, NPT, SKEW = 4, 6, 3

                            def unit_front(i, it):
                                pi, d, nblk, hl, gh, u, r_, n, accview, last = it
                                cc = hl // 2
                                qs = d * 128 * n + r_ - HALF
                                qap = qTm[:, hl, qs:qs + d * 127 + 1:d]
                                sti, pti = i % NST, i % NPT
                                for j, b in enumerate((n - 1, n)):
                                    ks = d * 128 * b + r_
                                    MM(pST[sti][:, j * 128:(j + 1) * 128], kT[:, cc, ks:ks + d * 127 + 1:d], qap, True, True,
                                       ["kT", "qTm"], [("pST", sti)])
                                ACT(EX[pti][:], pST[sti][:], AF.Exp, [("pST", sti)], [("EX", pti)])
                                TT("vector", PT[pti][:], EX[pti][:], etb[:, pi, hl, :], ALU.mult, [("EX", pti), "etb"], [("PT", pti)])

                            pend = []

                            def unit_back(i, it):
                                while pend:
                                    pend.pop(0)()
                                pi, d, nblk, hl, gh, u, r_, n, accview, last = it
                                cc, po = hl // 2, (hl % 2) * 64
                                go, pti = gh % 2, i % NPT
                                for j, b in enumerate((n - 1, n)):
                                    vi = vmap[(pi, r_, b)]
                                    prev_half = b < nblk // 2
                                    MM(pO[go][:, u * 128:(u + 1) * 128], vtok[:, vi, cc * 128:(cc + 1) * 128], PT[pti][:, j * 128:(j + 1) * 128],
                                       j == 0, j == 1, [("vtok", vi), ("PT", pti)], [("pO", go)])
                                    MM(pD[go][:, u * 128:(u + 1) * 128], (pones_b if prev_half else ones_b)[:], PT[pti][:, j * 128:(j + 1) * 128],
                                       j == 0, j == 1, ["pones_b", "ones_b", ("PT", pti)], [("pD", go)])
                                if not last:
                                    return
                                ao = accview(accO[po:po + 64, cc, :])
                                ad = accview(accD[po:po + 64, cc, :])
                                so = pO[go][po:po + 64, :]
                                sd = pD[go][po:po + 64, :]
                                if d == 16:
                                    so = so.rearrange("p (u q) -> p u q", u=4)
                                    sd = sd.rearrange("p (u q) -> p u q", u=4)
                                if pi == 0:
                                    ACT(ao, so, AF.Copy, [("pO", go)], [("accO", hl)])
                                    pend.append(lambda: ACT(ad, sd, AF.Copy, [("pD", go)], [("accD", hl)]))
                                else:
                                    TT("vector", ao, so, ao, ALU.add, [("pO", go), ("accO", hl)], [("accO", hl)])
                                    pend.append(lambda: TT("vector", ad, sd, ad, ALU.add, [("pD", go), ("accD", hl)], [("accD", hl)]))

                            for i in range(len(items) + SKEW if items else 0):
                                if i < len(items):
                                    unit_front(i, items[i])
                                if i >= SKEW:
                                    unit_back(i - SKEW, items[i - SKEW])
                            while pend:
                                pend.pop(0)()
                            acck = [("accO", h_) for h_ in range(4)] + [("accD", h_) for h_ in range(4)]
                            for cc in range(2 if ALV >= 9 else 0):
                                REC(accD[:, cc, :], accD[:, cc, :], acck, acck)
                                TT("vector", accO[:, cc, :], accO[:, cc, :], accD[:, cc, :], ALU.mult, acck, acck)
                                ACT(yT[:, pas * 2 + cc, :], accO[:, cc, :], AF.Copy, acck, [("yT", "a", pas)])
                                TT("vector", sqa[:, cc, :], accO[:, cc, :], accO[:, cc, :], ALU.mult, acck, ["sqa"])
                            if ALV >= 9:
                                S.barrier()
                                psa = pST[0]
                            for ti in range(16 if ALV >= 9 else 0):
                                MMG(psa[:, ti:ti + 1], [(sqa[:, cc, ti * 128:(ti + 1) * 128], onescol_b[:, 0:1]) for cc in range(2)],
                                    ["sqa", "onescol_b"], ["psa"])
                            if ALV >= 9:
                                TT("vector", ssq[:, 0:16], psa[:, 0:16], ssq[:, 0:16], ALU.add, ["psa", "ssq"], ["ssq"])

            if debug:
                with contextlib.ExitStack() as dd:
                    dd.callback(S.barrier)
                    dtmp = sb(dd, "dtmp", [128, 16, HALF])
                    CP("vector", dtmp[:], yT[:], [("yT", "l", w_) for w_ in range(HALF // LW, 2 * HALF // LW)] + [("yT", "a", p_) for p_ in range(4)], ["dtmp"])
                    DMA("sync", dbg["yT"], dtmp[:], "dbg", ["dtmp"], [], final=True)
                    DMA("sync", dbg["ssq"], ssq[:], "dbg", ["ssq"], [], final=True)

            yT_keys = [("yT", "l", w_) for w_ in range(HALF // LW, 2 * HALF // LW)] + [("yT", "a", p_) for p_ in range(4)]

            if stop_after in ("lru", "mixer", "a1", "a2", "a2p0", "a2p1", "a2p2", "a3", "a3p0"):
                pass
            else:
                with contextlib.ExitStack() as ph:
                    ph.callback(S.barrier)
                    wo = sb(ph, "wo", [128, 16, D], BF16)
                    gcat = sb(ph, "gcat", [128, 16])
                    g1 = sb(ph, "g1", [128, D])
                    b1 = sb(ph, "b1", [128, D])
                    wr = sb(ph, "wr", [128, 16, 36])
                    brt = sb(ph, "brt", [128, 36])
                    tri = sb(ph, "tri", [128, 128])
                    ebase = sb(ph, "ebase", [128, NE])
                    cm = sb(ph, "cm", [128, NE])
                    rstd = sb(ph, "rstd", [128, 32])
                    DMA("sync", gcat[:], gcat_d, "k3", [], ["gcat"])
                    DMA("sync", g1[:], ln1g_d, "k4", [], ["g1"])
                    DMA("sync", b1[:], ln1b_d, "k5", [], ["b1"])
                    DMA("sync", wr[:], wr_d.rearrange("(k p) n -> p k n", p=128), "k6", [], ["wr"])
                    DMA("sync", brt[:], br_d, "k7", [], ["brt"])
                    DMA("sync", tri[:], tri_d, "k8", [], ["tri"])
                    DMA("sync", ebase[:], ebase_d, "k9", [], ["ebase"])
                    MS("vector", cm[:], 0.0, ["cm"])
                    TS("vector", rstd[:], ssq[:], 1.0 / 1024.0, RMS_EPS, ALU.mult, ALU.add, ["ssq"], ["rstd"])
                    ACT(rstd[:], rstd[:], AF.Sqrt, ["rstd"], ["rstd"])
                    REC(rstd[:], rstd[:], ["rstd"], ["rstd"])
                    with contextlib.ExitStack() as wp:
                        wp.callback(S.barrier)
                        stg = [sb(wp, f"ostg{i}", [128, D]) for i in range(2)]
                        w_out_v = w_out.rearrange("(k p) n -> p k n", p=128)
                        for k in range(16):
                            s_ = k % 2
                            DMA(hwq(), stg[s_][:], w_out_v[:, k, :], f"ow{s_}", [], [f"ostg{s_}"])
                            if k % 2:
                                ACT(wo[:, k, :], stg[s_][:], AF.Copy, [f"ostg{s_}", "gcat"], [("wo", k)], scale=gcat[:, k:k + 1])
                            else:
                                TS("vector", wo[:, k, :], stg[s_][:], gcat[:, k:k + 1], None, ALU.mult, None,
                                   [f"ostg{s_}", "gcat"], [("wo", k)])
                    with contextlib.ExitStack() as tl:
                        tl.callback(S.barrier)
                        zb = [sb(tl, f"zb{i}", [128, D]) for i in range(2)]
                        x1 = sb(tl, "x1", [128, D])
                        x1b = sb(tl, "x1b", [128, D], BF16)
                        x1T = sb(tl, "x1T", [128, 16, 128])
                        st6s = [sb(tl, f"st6{i}", [128, 4, 6]) for i in range(2)]
                        mv = sb(tl, "mv", [128, 2])
                        sm = sb(tl, "sm", [128, 16])
                        lg = sb(tl, "lg", [128, 36])
                        lm = sb(tl, "lm", [128, NE])
                        lm2 = sb(tl, "lm2", [128, NE])
                        mk1 = sb(tl, "mk1", [128, NE])
                        mk2 = sb(tl, "mk2", [128, NE])
                        mk = sb(tl, "mk", [128, NE])
                        slot = sb(tl, "slot", [128, NE])
                        ovf = sb(tl, "ovf", [128, NE])
                        g4 = sb(tl, "g4", [128, 4])
                        oh4 = sb(tl, "oh4", [128, 4])
                        slf = sb(tl, "slf", [128, 2])
                        pA = [ps(tl, f"pA{i}", [128, 512]) for i in range(2)]
                        pL = [ps(tl, f"pL{i}", [128, 512]) for i in range(2)]
                        pT = [ps(tl, f"pT{i}", [128, 512]) for i in range(2)]
                        pR = ps(tl, "pR", [128, 36])
                        pC = ps(tl, "pC", [128, NE])
                        wo_keys = [("wo", k) for k in range(16)]
                        def part_a(ti):
                            z = zb[ti % 2]
                            zk = f"zb{ti % 2}"
                            tk0 = ti * 128
                            st6 = st6s[ti % 2]
                            s6k = f"st6{ti % 2}"
                            DMA(hwq(), z[:], xtok[tk0:tk0 + 128, :], f"xt{ti % 2}", [], [zk])
                            ACT(z[:], z[:], AF.Copy, [zk], [zk], scale=ALPHA)
                            for nb_ in range(4):
                                b_ = (ti * 4 + nb_) % 2
                                MMG(pA[b_][:], [(yT[:, k, tk0:tk0 + 128], wo[:, k, nb_ * 512:(nb_ + 1) * 512]) for k in range(8)],
                                    yT_keys + wo_keys, [("pA", b_)])
                                MMG(pL[b_][:], [(yT[:, k, tk0:tk0 + 128], wo[:, k, nb_ * 512:(nb_ + 1) * 512]) for k in range(8, 16)],
                                    yT_keys + wo_keys, [("pL", b_)])
                                zs = z[:, nb_ * 512:(nb_ + 1) * 512]
                                STT(zs, pA[b_][:], rstd[:, ti:ti + 1], zs, ALU.mult, ALU.add, [("pA", b_), "rstd", zk], [zk])
                                STT(zs, pL[b_][:], rstd[:, 16 + ti:17 + ti], zs, ALU.mult, ALU.add, [("pL", b_), "rstd", zk], [zk])
                                BNS(st6[:, nb_, :], zs, [zk], [s6k])
                                yield
                        def part_b(ti):
                            z = zb[ti % 2]
                            zk = f"zb{ti % 2}"
                            tk0 = ti * 128
                            st6 = st6s[ti % 2]
                            s6k = f"st6{ti % 2}"
                            BNA(mv[:], st6[:].rearrange("p a b -> p (a b)"), [s6k], ["mv"])
                            TS("vector", sm[:, 0:1], mv[:, 1:2], LN_EPS, None, ALU.add, None, ["mv"], ["sm"])
                            ACT(sm[:, 0:1], sm[:, 0:1], AF.Sqrt, ["sm"], ["sm"])
                            REC(sm[:, 0:1], sm[:, 0:1], ["sm"], ["sm"])
                            STT(sm[:, 1:2], mv[:, 0:1], -1.0, sm[:, 0:1], ALU.mult, ALU.mult, ["mv", "sm"], ["sm"])
                            ACT(x1[:], z[:], AF.Identity, [zk, "sm"], ["x1"], bias=sm[:, 1:2], scale=sm[:, 0:1])
                            TT("vector", x1[:], x1[:], g1[:], ALU.mult, ["x1", "g1"], ["x1"])
                            TT("vector", x1[:], x1[:], b1[:], ALU.add, ["x1", "b1"], ["x1"])
                            DMA("sync", x1s[tk0:tk0 + 128, :], x1[:], "x1st", ["x1"], [("x1s", ti)])
                            if debug:
                                DMA("sync", dbg["x1"][tk0:tk0 + 128, :], x1[:], "dbg", ["x1"], [], final=True)
                            ACT(x1b[:], x1[:], AF.Copy, ["x1"], ["x1b"])
                            yield
                            for k4 in range(4):
                                pt = pT[k4 % 2]
                                for kk in range(4):
                                    k = k4 * 4 + kk
                                    TR(pt[:, kk * 128:(kk + 1) * 128], x1[:, k * 128:(k + 1) * 128], ident_f[:], ["x1", "ident_f"], [("pT", k4 % 2)])
                                if k4 % 2:
                                    ACT(x1T[:, k4 * 4:(k4 + 1) * 4, :], pt[:].rearrange("p (a b) -> p a b", a=4), AF.Copy, [("pT", k4 % 2)], ["x1T"])
                                else:
                                    CP("vector", x1T[:, k4 * 4:(k4 + 1) * 4, :], pt[:].rearrange("p (a b) -> p a b", a=4), [("pT", k4 % 2)], ["x1T"])
                            MMG(pR[:], [(x1T[:, k, :], wr[:, k, :]) for k in range(16)], ["x1T", "wr"], ["pR"])
                            TT("vector", lg[:], pR[:], brt[:], ALU.add, ["pR", "brt"], ["lg"])
                            yield
                            RED(sm[:, 2:3], lg[:, 0:4], ALU.max, ["lg"], ["sm"])
                            TS("vector", sm[:, 3:4], sm[:, 2:3], -1.0, None, ALU.mult, None, ["sm"], ["sm"])
                            ACT(g4[:], lg[:, 0:4], AF.Exp, ["lg", "sm"], ["g4", "sm"], bias=sm[:, 3:4], accum=sm[:, 4:5])
                            REC(sm[:, 5:6], sm[:, 4:5], ["sm"], ["sm"])
                            TS("vector", oh4[:], lg[:, 0:4], sm[:, 2:3], None, ALU.is_equal, None, ["lg", "sm"], ["oh4"])
                            TS("vector", oh4[:], oh4[:], -1.0, 1.0e30, ALU.add, ALU.mult, ["oh4"], ["oh4"])
                            for g in range(4):
                                TS("vector", lm[:, 8 * g:8 * g + 8], lg[:, 4 + 8 * g:12 + 8 * g], oh4[:, g:g + 1], None, ALU.add, None,
                                   ["lg", "oh4"], ["lm"])
                            RED(sm[:, 6:7], lm[:], ALU.max, ["lm"], ["sm"])
                            TS("vector", mk1[:], lm[:], sm[:, 6:7], None, ALU.is_equal, None, ["lm", "sm"], ["mk1"])
                            STT(lm2[:], mk1[:], -1.0e30, lm[:], ALU.mult, ALU.add, ["mk1", "lm"], ["lm2"])
                            RED(sm[:, 7:8], lm2[:], ALU.max, ["lm2"], ["sm"])
                            TS("vector", mk2[:], lm2[:], sm[:, 7:8], None, ALU.is_equal, None, ["lm2", "sm"], ["mk2"])
                            TT("vector", sm[:, 8:9], sm[:, 6:7], sm[:, 7:8], ALU.subtract, ["sm"], ["sm"])
                            ACT(sm[:, 9:10], sm[:, 8:9], AF.Sigmoid, ["sm"], ["sm"])
                            ACT(sm[:, 10:11], sm[:, 8:9], AF.Sigmoid, ["sm"], ["sm"], scale=-1.0)
                            TT("vector", wts[:, ti, 0:1], sm[:, 9:10], sm[:, 5:6], ALU.mult, ["sm"], ["wts"])
                            TT("vector", wts[:, ti, 1:2], sm[:, 10:11], sm[:, 5:6], ALU.mult, ["sm"], ["wts"])
                            TT("vector", mk[:], mk1[:], mk2[:], ALU.add, ["mk1", "mk2"], ["mk"])
                            yield
                            MM(pC[:], tri[:], mk[:], True, False, ["tri", "mk"], ["pC"])
                            MM(pC[:], ones_f[:], cm[:], False, True, ["ones_f", "cm"], ["pC"])
                            TS("vector", ovf[:], pC[:], CAP - 0.5, BIG, ALU.is_ge, ALU.mult, ["pC"], ["ovf"])
                            TT("vector", slot[:], pC[:], ebase[:], ALU.add, ["pC", "ebase"], ["slot"])
                            TT("vector", slot[:], slot[:], ovf[:], ALU.add, ["slot", "ovf"], ["slot"])
                            TT("vector", cm[:], cm[:], mk[:], ALU.add, ["cm", "mk"], ["cm"])
                            TT("vector", mk1[:], mk1[:], slot[:], ALU.mult, ["mk1", "slot"], ["mk1"])
                            TT("vector", mk2[:], mk2[:], slot[:], ALU.mult, ["mk2", "slot"], ["mk2"])
                            RED(slf[:, 0:1], mk1[:], ALU.add, ["mk1"], ["slf"])
                            RED(slf[:, 1:2], mk2[:], ALU.add, ["mk2"], ["slf"])
                            CP("vector", slots_i[:, ti, :], slf[:], ["slf"], [("slots", ti)])
                            if debug:
                                CP("vector", sm[:, 12:14], slf[:], ["slf"], ["sm"])
                                CP("vector", sm[:, 14:16], wts[:, ti, :], ["wts", "sm"], ["sm"])
                                DMA("sync", dbg["rt"][:, ti, :], sm[:, 12:16], "dbg", ["sm"], [], final=True)
                            for a_ in range(2):
                                SCAT(xs_d, slots_i[:, ti, a_:a_ + 1], x1b[:], "scat", ["x1b", ("slots", ti)], [("xs", ti, a_)])

                        for _ in part_a(0):
                            pass
                        for ti in range(1, 16):
                            gb = part_b(ti - 1)
                            for _ in part_a(ti):
                                next(gb, None)
                            for _ in gb:
                                pass
                        for _ in part_b(15):
                            pass

        if stop_after in ("lru", "mixer", "ln1", "a1", "a2", "a2p0", "a2p1", "a2p2", "a3", "a3p0"):
            with contextlib.ExitStack() as dd:
                dd.callback(S.barrier)
                zt = sb(dd, "zt", [128, D])
                MS("vector", zt[:], 0.0, ["zt"])
                for ti in range(16):
                    DMA("sync", out_d[ti * 128:(ti + 1) * 128, :], zt[:], "outst", ["zt"], [], final=True)
        else:
            with contextlib.ExitStack() as ph:
                ph.callback(S.barrier)
                w1b = [sb(ph, f"w1b{i}", [128, 16, DE], BF16) for i in range(2)]
                w3b = [sb(ph, f"w3b{i}", [128, 16, DE], BF16) for i in range(2)]
                w2b = [sb(ph, f"w2b{i}", [128, 4, D], BF16) for i in range(2)]
                stg = [sb(ph, f"estg{i}", [128, 2048]) for i in range(NSTG)]
                xe = sb(ph, "xe", [128, 2, D], BF16)
                xeT = sb(ph, "xeT", [128, 16, CAP], BF16)
                h1 = sb(ph, "h1", [128, CAP])
                hT = sb(ph, "hT", [128, 4, CAP], BF16)
                yo = [sb(ph, f"yo{i}", [128, D]) for i in range(2)]
                ptr = [ps(ph, f"etr{i}", [128, 512], BF16) for i in range(2)]
                ph1 = [ps(ph, f"ph1{i}", [128, CAP]) for i in range(2)]
                ph3 = [ps(ph, f"ph3{i}", [128, CAP]) for i in range(2)]
                py = [ps(ph, f"py{i}", [128, 512]) for i in range(2)]
                sctr = [0]
                cast_engs = ["vector", "scalar", "vector", "gpsimd", "scalar", "vector", "scalar", "vector", "gpsimd", "vector", "scalar", "gpsimd"]

                def load_expert(e_):
                    s_ = e_ % 2
                    for wt, src, nk in ((w1b[s_], w1_d[e_], 16), (w3b[s_], w3_d[e_], 16)):
                        for k4 in range(4):
                            si = sctr[0] % NSTG
                            sctr[0] += 1
                            DMA(hwq(), stg[si][:], src[:, k4 * 4 * DE:(k4 + 1) * 4 * DE], f"ew{si}", [], [f"estg{si}"])
                            ce = cast_engs[sctr[0] % 12]
                            dst = wt[:, k4 * 4:(k4 + 1) * 4, :].rearrange("p k n -> p (k n)")
                            if ce == "scalar":
                                ACT(dst, stg[si][:], AF.Copy, [f"estg{si}"], [(wt.name, k4)])
                            else:
                                CP(ce, dst, stg[si][:], [f"estg{si}"], [(wt.name, k4)])
                            yield
                    src = w2_d[e_].rearrange("(k p) n -> p k n", p=128)
                    for k in range(4):
                        si = sctr[0] % NSTG
                        sctr[0] += 1
                        DMA(hwq(), stg[si][:], src[:, k, :], f"ew{si}", [], [f"estg{si}"])
                        ce = cast_engs[sctr[0] % 12]
                        if ce == "scalar":
                            ACT(w2b[s_][:, k, :], stg[si][:], AF.Copy, [f"estg{si}"], [(w2b[s_].name, k)])
                        else:
                            CP(ce, w2b[s_][:, k, :], stg[si][:], [f"estg{si}"], [(w2b[s_].name, k)])
                        yield

                ldr = [iter(())]

                def pump(n):
                    for _ in range(n):
                        next(ldr[0], None)

                for _ in load_expert(0):
                    pass
                for e_ in range(NE):
                    s_ = e_ % 2
                    ldr[0] = load_expert(e_ + 1) if e_ + 1 < NE else iter(())
                    r0 = e_ * CAP
                    DMA("sync", xe[:], xs_d[r0:r0 + CAP, :].rearrange("(b p) d -> p b d", p=128), "xe", [("xs", t_, a2) for t_ in range(16) for a2 in range(2)], ["xe"])
                    pump(2)
                    for b in range(2):
                        for k4 in range(4):
                            pt = ptr[(b * 4 + k4) % 2]
                            pk = ("etr", (b * 4 + k4) % 2)
                            for kk in range(4):
                                k = k4 * 4 + kk
                                TR(pt[:, kk * 128:(kk + 1) * 128], xe[:, b, k * 128:(k + 1) * 128], ident_b[:], ["xe", "ident_b"], [pk])
                            dst = xeT[:, k4 * 4:(k4 + 1) * 4, b * 128:(b + 1) * 128]
                            srcp = pt[:].rearrange("p (a q) -> p a q", a=4)
                            if k4 % 2:
                                ACT(dst, srcp, AF.Copy, [pk], ["xeT"])
                            else:
                                CP("vector", dst, srcp, [pk], ["xeT"])
                            pump(1)
                    w1k = [(w1b[s_].name, k4) for k4 in range(4)]
                    w3k = [(w3b[s_].name, k4) for k4 in range(4)]
                    w2k = [(w2b[s_].name, k) for k in range(4)]
                    for m in range(4):
                        b_ = m % 2
                        MMG(ph1[b_][:], [(w1b[s_][:, k, m * 128:(m + 1) * 128], xeT[:, k, :]) for k in range(16)],
                            w1k + ["xeT"], [("ph1", b_)])
                        MMG(ph3[b_][:], [(w3b[s_][:, k, m * 128:(m + 1) * 128], xeT[:, k, :]) for k in range(16)],
                            w3k + ["xeT"], [("ph3", b_)])
                        ACT(h1[:], ph1[b_][:], AF.Silu, [("ph1", b_)], ["h1"])
                        TT("vector", hT[:, m, :], h1[:], ph3[b_][:], ALU.mult, ["h1", ("ph3", b_)], ["hT"])
                        pump(2)
                    for b in range(2):
                        yb_ = yo[b]
                        for n_ in range(4):
                            pb = (b * 4 + n_) % 2
                            MMG(py[pb][:], [(hT[:, m, b * 128:(b + 1) * 128], w2b[s_][:, m, n_ * 512:(n_ + 1) * 512]) for m in range(4)],
                                w2k + ["hT"], [("py", pb)])
                            if n_ % 2:
                                ACT(yb_[:, n_ * 512:(n_ + 1) * 512], py[pb][:], AF.Copy, [("py", pb)], [yb_.name])
                            else:
                                CP("vector", yb_[:, n_ * 512:(n_ + 1) * 512], py[pb][:], [("py", pb)], [yb_.name])
                            pump(1)
                        DMA("scalar", ys_d[r0 + b * 128:r0 + (b + 1) * 128, :], yb_[:], "yst", [yb_.name], [("ys", e_, b)])
                    pump(99)

            with contextlib.ExitStack() as ph:
                ph.callback(S.barrier)
                g2 = sb(ph, "g2", [128, D])
                b2 = sb(ph, "b2", [128, D])
                ya = [sb(ph, f"ya{i}", [128, D]) for i in range(2)]
                yb = [sb(ph, f"yb{i}", [128, D]) for i in range(2)]
                xz = [sb(ph, f"xz{i}", [128, D]) for i in range(2)]
                oo = [sb(ph, f"oo{i}", [128, D]) for i in range(2)]
                st6 = sb(ph, "st6b", [128, 4, 6])
                mv = sb(ph, "mvb", [128, 2])
                sm = sb(ph, "smb", [128, 4])
                x1s_keys = [("x1s", t_) for t_ in range(16)]
                ys_keys = [("ys", e2, b2_) for e2 in range(NE) for b2_ in range(2)]
                DMA("sync", g2[:], ln2g_d, "k10", [], ["g2"])
                DMA("sync", b2[:], ln2b_d, "k11", [], ["b2"])
                for ti in range(16):
                    i_ = ti % 2
                    tk0 = ti * 128
                    MS("vector", ya[i_][:], 0.0, [f"ya{i_}"])
                    MS("vector", yb[i_][:], 0.0, [f"yb{i_}"])
                    for a_, dst, dk in ((0, ya[i_], f"ya{i_}"), (1, yb[i_], f"yb{i_}")):
                        GATH(dst[:], ys_d, slots_i[:, ti, a_:a_ + 1], f"gath{i_}{a_}", ys_keys + [("slots", ti)], [dk])
                    DMA(hwq(), xz[i_][:], x1s[tk0:tk0 + 128, :], f"xz{i_}", x1s_keys, [f"xz{i_}"])
                    ACT(xz[i_][:], xz[i_][:], AF.Copy, [f"xz{i_}"], [f"xz{i_}"], scale=ALPHA)
                    STT(xz[i_][:], ya[i_][:], wts[:, ti, 0:1], xz[i_][:], ALU.mult, ALU.add, [f"ya{i_}", "wts", f"xz{i_}"], [f"xz{i_}"])
                    STT(xz[i_][:], yb[i_][:], wts[:, ti, 1:2], xz[i_][:], ALU.mult, ALU.add, [f"yb{i_}", "wts", f"xz{i_}"], [f"xz{i_}"])
                    for nb_ in range(4):
                        BNS(st6[:, nb_, :], xz[i_][:, nb_ * 512:(nb_ + 1) * 512], [f"xz{i_}"], ["st6b"])
                    BNA(mv[:], st6[:].rearrange("p a b -> p (a b)"), ["st6b"], ["mvb"])
                    TS("vector", sm[:, 0:1], mv[:, 1:2], LN_EPS, None, ALU.add, None, ["mvb"], ["smb"])
                    ACT(sm[:, 0:1], sm[:, 0:1], AF.Sqrt, ["smb"], ["smb"])
                    REC(sm[:, 0:1], sm[:, 0:1], ["smb"], ["smb"])
                    STT(sm[:, 1:2], mv[:, 0:1], -1.0, sm[:, 0:1], ALU.mult, ALU.mult, ["mvb", "smb"], ["smb"])
                    ACT(oo[i_][:], xz[i_][:], AF.Identity, [f"xz{i_}", "smb"], [f"oo{i_}"], bias=sm[:, 1:2], scale=sm[:, 0:1])
                    TT("vector", oo[i_][:], oo[i_][:], g2[:], ALU.mult, [f"oo{i_}", "g2"], [f"oo{i_}"])
                    TT("vector", oo[i_][:], oo[i_][:], b2[:], ALU.add, [f"oo{i_}", "b2"], [f"oo{i_}"])
                    DMA("sync", out_d[tk0:tk0 + 128, :], oo[i_][:], "outst", [f"oo{i_}"], [], final=True)

        S.emit()
    return nc


def _host_consts():
    slopes = np.exp2(-8.0 * np.arange(1, NH + 1, dtype=np.float64) / NH)
    i = np.arange(128)[:, None]
    q = np.arange(128)[None, :]
    etab = np.zeros((128, 3, NH, 256), np.float32)
    for pi, d in enumerate(PATTERNS):
        for h in range(NH):
            dist0 = q + 128 - i
            dist1 = q - i
            etab[:, pi, h, 0:128] = np.where(dist0 <= 128, np.exp(-slopes[h] * d * np.minimum(dist0, 128)), 0.0)
            etab[:, pi, h, 128:256] = np.where(dist1 >= 0, np.exp(-slopes[h] * d * np.maximum(dist1, 0)), 0.0)
    ident = np.eye(128, dtype=np.float32)
    tri = (np.arange(128)[:, None] < np.arange(128)[None, :]).astype(np.float32)
    ebase = np.tile((np.arange(NE, dtype=np.float32) * CAP)[None, :], (128, 1))
    return etab, ident, tri, ebase


def _chunked(v):
    return np.ascontiguousarray(np.asarray(v, np.float32).reshape(8, 128).T)


def make_in_maps(x, w_in, conv_w, conv_b, lru_wa, lru_ba, lru_wx, lru_bx, lru_lambda, attn_norm_g, lru_norm_g,
                 w_out, ln1_g, ln1_b, router_grp_w, router_grp_b, router_exp_w, router_exp_b, w1, w3, w2,
                 ln2_g, ln2_b):
    f = lambda a: np.ascontiguousarray(np.asarray(a, np.float32))
    x = f(x)
    etab, ident, tri, ebase = _host_consts()
    convw = np.ascontiguousarray(f(conv_w)[0].reshape(4, 8, 128).transpose(2, 1, 0))

    def bd(wg):
        wg = f(wg)[0]
        o = np.zeros((128, 8, 128), np.float32)
        for n in range(16):
            c, hlf = n // 2, n % 2
            o[hlf * 64:(hlf + 1) * 64, c, hlf * 64:(hlf + 1) * 64] = wg[n]
        return o
    gcat = np.concatenate([_chunked(f(attn_norm_g)[0]), _chunked(f(lru_norm_g)[0])], axis=1)
    bc = lambda v: np.ascontiguousarray(np.broadcast_to(f(v).reshape(1, -1), (128, f(v).size)))
    wr = np.ascontiguousarray(np.concatenate([f(router_grp_w)[0], f(router_exp_w)[0]], axis=1))
    br = bc(np.concatenate([f(router_grp_b)[0], f(router_exp_b)[0]]))
    pk = lambda w: np.ascontiguousarray(f(w)[0].reshape(NE, 16, 128, DE).transpose(0, 2, 1, 3)).reshape(NE, 128, 16 * DE)
    shared = dict(
        w_in=f(w_in)[0], w_out=f(w_out)[0], convw=convw, convb=_chunked(f(conv_b)[0]),
        lba=_chunked(f(lru_ba)[0].reshape(-1)), lbx=_chunked(f(lru_bx)[0].reshape(-1)), lam=_chunked(f(lru_lambda)[0]),
        wbda=bd(lru_wa), wbdx=bd(lru_wx), gcat=gcat,
        ln1g=bc(f(ln1_g)[0]), ln1b=bc(f(ln1_b)[0]), ln2g=bc(f(ln2_g)[0]), ln2b=bc(f(ln2_b)[0]),
        wr=wr, br=br, w1=pk(w1), w3=pk(w3), w2=f(w2)[0],
        etab=etab, ident=ident, tri=tri, ebase=ebase,
    )
    maps = []
    for c in range(8):
        b, h = c // 2, c % 2
        own = x[b, h * HALF:(h + 1) * HALF]
        prev = x[b, 0:HALF] if h == 1 else np.zeros((HALF, D), np.float32)
        xfull = np.concatenate([prev, own], axis=0)
        xT = np.ascontiguousarray(xfull.reshape(2 * HALF // LW, LW, 16, 128).transpose(0, 3, 2, 1)).reshape(2 * HALF // LW, 128, 16 * LW)
        m = dict(shared)
        m["xT"] = xT
        m["xtok"] = np.ascontiguousarray(own)
        m["flag"] = np.full((128, 1), float(h), np.float32)
        maps.append(m)
    return maps


def kernel(**inputs):
    nc = build_program()
    maps = make_in_maps(**inputs)
    res = run_bass_kernel_spmd(nc, maps, core_ids=list(range(8)))
    out = np.zeros((NB, SEQ, D), np.float32)
    for c in range(8):
        b, h = c // 2, c % 2
        out[b, h * HALF:(h + 1) * HALF] = np.asarray(res.results[c]["out"], np.float32)
    return out
```

```python
import contextlib
import numpy as np
import ml_dtypes
import concourse.bass as bass
import concourse.mybir as mybir
from concourse.alu_op_type import AluOpType as ALU
from concourse.bass_utils import run_bass_kernel_spmd

F32 = mybir.dt.float32
BF16 = mybir.dt.bfloat16
I32 = mybir.dt.int32
AF = mybir.ActivationFunctionType
AX = mybir.AxisListType

D = 2048
SEQ = 4096
NB = 4
HALF = 2048
NH = 16
DLRU = 1024
DPROJ = 5120
NE = 32
DE = 512
CAP = 256
ALPHA = 2.0 ** 0.25
LN_EPS = 1e-5
RMS_EPS = 1e-6
PATTERNS = (1, 4, 16)
WIN = 256
LW = 128
AW = 128
BIG = 1.0e6
NSTG = 8

ENGS = ("sync", "scalar", "vector", "gpsimd", "tensor")


class Sched:
    def __init__(self, nc, self_sync=True):
        self.nc = nc
        self.self_sync = self_sync
        self.q = {e: [] for e in ENGS}
        self.cnt = {}
        self.waited = {e: {} for e in ENGS}
        self.last_w = {}
        self.readers = {}
        self.sems = {}
        self.semkeys = []
        self.final = []
        self.bar = {}

    def barrier(self):
        self.bar = dict(self.cnt)

    def _semkey(self, k):
        if k not in self.cnt:
            self.cnt[k] = 0
            self.semkeys.append(k)
        return k

    def _deps(self, eng, r, w):
        deps = {}

        def add(t):
            if t is None:
                return
            k, v = t
            if deps.get(k, 0) < v:
                deps[k] = v
        for key in r:
            add(self.last_w.get(key))
        for key in w:
            add(self.last_w.get(key))
            for t in self.readers.get(key, ()):
                add(t)
        for k, v in self.bar.items():
            if v:
                add((k, v))
        out = []
        for k, v in deps.items():
            if k == eng and (eng != "vector" or not self.self_sync):
                continue
            if self.waited[eng].get(k, 0) >= v:
                continue
            self.waited[eng][k] = v
            out.append((k, v))
        return out

    def _record(self, ticket, r, w):
        for key in w:
            self.last_w[key] = ticket
            self.readers[key] = []
        for key in r:
            self.readers.setdefault(key, []).append(ticket)

    def op(self, eng, fn, r=(), w=()):
        waits = self._deps(eng, r, w)
        k = self._semkey(eng)
        self.cnt[k] += 1
        ticket = (k, self.cnt[k])
        self.q[eng].append((fn, waits, k, 1))
        self._record(ticket, r, w)
        return ticket

    def dma(self, eng, fn, sem, r=(), w=(), final=False):
        waits = self._deps(eng, r, w)
        k = self._semkey("dma:" + sem)
        self.cnt[k] += 16
        ticket = (k, self.cnt[k])
        self.q[eng].append((fn, waits, k, 16))
        self._record(ticket, r, w)
        if final:
            self.final.append(ticket)
        return ticket

    def emit(self):
        nc = self.nc
        with contextlib.ExitStack() as st:
            for k in self.semkeys:
                self.sems[k] = st.enter_context(nc.semaphore(k.replace(":", "_")))
            fin = {}
            for k, v in self.final:
                fin[k] = max(fin.get(k, 0), v)
            block = st.enter_context(nc.Block())
            for e in ENGS:
                ops = self.q[e]
                extra = fin if e == "sync" else {}
                if not ops and not extra:
                    continue

                def body(eh, ops=ops, extra=extra):
                    for fn, waits, k, inc in ops:
                        for wk, wv in waits:
                            eh.wait_ge(self.sems[wk], wv)
                        fn(eh).then_inc(self.sems[k], inc)
                    for wk, wv in extra.items():
                        eh.wait_ge(self.sems[wk], wv)

                getattr(block, e)(body)


def build_program(stop_after=None, debug=False, skip_lru=False):
    nc = bass.Bass("TRN2", target_bir_lowering=False)

    def din(name, shape, dt=F32):
        return nc.dram_tensor(name, list(shape), dt, kind="ExternalInput").ap()

    xT = din("xT", [2 * HALF // LW, 128, 16 * LW])
    xtok = din("xtok", [HALF, D])
    w_in = din("w_in", [D, DPROJ])
    w_out = din("w_out", [D, D])
    wqkv_d = din("wqkv", [4, 128, 16 * 768])
    convw = din("convw", [128, 8, 4])
    convb = din("convb", [128, 8])
    ba_d = din("lba", [128, 8])
    bx_d = din("lbx", [128, 8])
    lam_d = din("lam", [128, 8])
    wbda_d = din("wbda", [128, 8, 128])
    wbdx_d = din("wbdx", [128, 8, 128])
    gcat_d = din("gcat", [128, 16])
    ln1g_d = din("ln1g", [128, D])
    ln1b_d = din("ln1b", [128, D])
    ln2g_d = din("ln2g", [128, D])
    ln2b_d = din("ln2b", [128, D])
    wr_d = din("wr", [D, 36])
    br_d = din("br", [128, 36])
    ne_in = NE if stop_after is None else 1
    w1_d = din("w1", [ne_in, 128, 16 * DE])
    w3_d = din("w3", [ne_in, 128, 16 * DE])
    w2_d = din("w2", [ne_in, DE, D])
    etab_d = din("etab", [128, 3, NH, 256])
    flag_d = din("flag", [128, 1])
    ident_d = din("ident", [128, 128])
    tri_d = din("tri", [128, 128])
    ebase_d = din("ebase", [128, NE])
    out_d = nc.dram_tensor("out", [HALF, D], F32, kind="ExternalOutput").ap()
    dbg = {}
    if debug:
        dbg["yT"] = nc.dram_tensor("dbg_yT", [128, 16, HALF], F32, kind="ExternalOutput").ap()
        dbg["ssq"] = nc.dram_tensor("dbg_ssq", [128, 32], F32, kind="ExternalOutput").ap()
        dbg["x1"] = nc.dram_tensor("dbg_x1", [HALF, D], F32, kind="ExternalOutput").ap()
        dbg["rt"] = nc.dram_tensor("dbg_rt", [128, 16, 4], F32, kind="ExternalOutput").ap()
    x1s = nc.dram_tensor("x1s", [HALF, D], F32).ap()
    xs_d = nc.dram_tensor("xs", [NE * CAP, D], BF16).ap()
    ys_d = nc.dram_tensor("ys", [NE * CAP, D], F32).ap()

    S = Sched(nc)
    dmaq = ["sync"]
    dctr = [0]

    def hwq():
        dctr[0] += 1
        return dmaq[dctr[0] % len(dmaq)]

    def MM(out, lhsT, rhs, start, stop, r, w):
        S.op("tensor", lambda e: e.matmul(out, lhsT, rhs, start=start, stop=stop), r, w)

    def MMG(out, pairs, r, w):
        n = len(pairs)
        for i, (lhsT, rhs) in enumerate(pairs):
            edge = i == 0 or i == n - 1
            MM(out, lhsT, rhs, i == 0, i == n - 1, r if edge else [], w if edge else [])

    def TR(out, in_, ident, r, w):
        S.op("tensor", lambda e: e.transpose(out, in_, ident), r, w)

    def ACT(out, in_, func, r, w, bias=None, scale=None, accum=None):
        kw = {}
        if bias is not None:
            kw["bias"] = bias
        if scale is not None:
            kw["scale"] = scale
        if accum is not None:
            kw["accum_out"] = accum
        S.op("scalar", lambda e: e.activation(out=out, in_=in_, func=func, **kw), r, w)

    def TS(eng, out, in0, s1, s2, op0, op1, r, w):
        if op1 is None:
            S.op(eng, lambda e: e.tensor_scalar(out, in0, s1, None, op0), r, w)
        else:
            S.op(eng, lambda e: e.tensor_scalar(out, in0, s1, s2, op0, op1), r, w)

    def STT(out, in0, scalar, in1, op0, op1, r, w):
        S.op("vector", lambda e: e.scalar_tensor_tensor(out, in0, scalar, in1, op0, op1), r, w)

    def TT(eng, out, in0, in1, op, r, w):
        S.op(eng, lambda e: e.tensor_tensor(out, in0, in1, op), r, w)

    def CP(eng, out, in_, r, w):
        S.op(eng, lambda e: e.tensor_copy(out=out, in_=in_), r, w)

    def MS(eng, ap, val, w):
        S.op(eng, lambda e: e.memset(ap, val), (), w)

    def REC(out, in_, r, w):
        S.op("vector", lambda e: e.reciprocal(out=out, in_=in_), r, w)

    def RED(out, in_, op, r, w):
        S.op("vector", lambda e: e.tensor_reduce(out=out, in_=in_, axis=AX.X, op=op), r, w)

    def BNS(out, in_, r, w):
        S.op("vector", lambda e: e.bn_stats(out=out, in_=in_), r, w)

    def BNA(out, in_, r, w):
        S.op("vector", lambda e: e.bn_aggr(out=out, in_=in_), r, w)

    def SCAN(out, d0, d1, init, r, w):
        S.op("vector", lambda e: e.tensor_tensor_scan(out=out, data0=d0, data1=d1, initial=init, op0=ALU.mult, op1=ALU.add), r, w)

    bcreg = {}

    def bc(e):
        if "r" not in bcreg:
            bcreg["r"] = e.to_reg(NE * CAP - 1)
        return bcreg["r"]

    def SCAT(dram, idx, src, sem, r, w):
        S.dma("gpsimd", lambda e: e.indirect_dma_start(out=dram, out_offset=bass.IndirectOffsetOnAxis(ap=idx, axis=0), in_=src, in_offset=None,
                                                       bounds_check=bc(e), oob_is_err=False), sem, r, w)

    def GATH(dst, dram, idx, sem, r, w):
        S.dma("gpsimd", lambda e: e.indirect_dma_start(out=dst, out_offset=None, in_=dram, in_offset=bass.IndirectOffsetOnAxis(ap=idx, axis=0),
                                                       bounds_check=bc(e), oob_is_err=False), sem, r, w)

    def DMA(eng, out, in_, sem, r, w, final=False):
        S.dma(eng, lambda e: e.dma_start(out=out, in_=in_), sem, r, w, final=final)

    with contextlib.ExitStack() as top:
        uniq = [0]

        def sb(st, name, shape, dt=F32):
            uniq[0] += 1
            return st.enter_context(nc.sbuf_tensor(f"s{uniq[0]}_{name}", list(shape), dt))

        def ps(st, name, shape, dt=F32):
            uniq[0] += 1
            return st.enter_context(nc.psum_tensor(f"p{uniq[0]}_{name}", list(shape), dt))

        ident_f = sb(top, "ident_f", [128, 128])
        ident_b = sb(top, "ident_b", [128, 128], BF16)
        flag = sb(top, "flag", [128, 1])
        ones_b = sb(top, "ones_b", [128, 128], BF16)
        pones_b = sb(top, "pones_b", [128, 128], BF16)
        ones_f = sb(top, "ones_f", [128, 128])
        onescol_b = sb(top, "onescol_b", [128, 1], BF16)
        ssq = sb(top, "ssq", [128, 32])
        slots_i = sb(top, "slots_i", [128, 16, 2], I32)
        wts = sb(top, "wts", [128, 16, 2])
        DMA("sync", ident_f[:], ident_d, "k1", (), ["ident_f"])
        DMA("sync", flag[:], flag_d, "k2", (), ["flag"])
        CP("vector", ident_b[:], ident_f[:], ["ident_f"], ["ident_b"])
        MS("vector", ones_b[:], 1.0, ["ones_b"])
        MS("vector", ones_f[:], 1.0, ["ones_f"])
        MS("vector", onescol_b[:], 1.0, ["onescol_b"])
        TS("vector", pones_b[:], ones_f[:], flag[:, 0:1], None, ALU.mult, None, ["ones_f", "flag"], ["pones_b"])
        MS("vector", ssq[:], 0.0, ["ssq"])

        with contextlib.ExitStack() as mix:
            mix.callback(S.barrier)
            yT = sb(mix, "yT", [128, 16, HALF], BF16)

            with contextlib.ExitStack() as ph:
                ph.callback(S.barrier)
                wl = sb(ph, "wl", [128, 16, 2048], BF16)
                cw = sb(ph, "cw", [128, 8, 4])
                cb = sb(ph, "cb", [128, 8])
                lba = sb(ph, "lba", [128, 8])
                lbx = sb(ph, "lbx", [128, 8])
                cl = sb(ph, "cl", [128, 8])
                cl2 = sb(ph, "cl2", [128, 8])
                wbda = sb(ph, "wbda", [128, 8, 128], BF16)
                wbdx = sb(ph, "wbdx", [128, 8, 128], BF16)
                stg = [sb(ph, "lstg0", [128, 2048])] * 2
                xw = [sb(ph, f"lxw{i}", [128, 16, LW], BF16) for i in range(2)]
                xr = sb(ph, "xr", [128, 8, LW + 3])
                xc2 = [sb(ph, f"xc{i}", [128, 8, LW]) for i in range(2)]
                xcb2 = [sb(ph, f"xcb{i}", [128, 8, LW], BF16) for i in range(2)]
                gr2 = [sb(ph, "gr0", [128, 8, LW])] * 2
                gi2 = [sb(ph, f"gi{i}", [128, 8, LW]) for i in range(2)]
                ga2 = [sb(ph, f"ga{i}", [128, 8, LW]) for i in range(2)]
                gs2 = [sb(ph, "gs0", [128, 8, LW])] * 2
                hh = [sb(ph, f"hh{i}", [128, 8, LW]) for i in range(2)]
                gg2 = [sb(ph, f"gg{i}", [128, 8, LW]) for i in range(2)]
                pxr = [ps(ph, f"pxr{i}", [128, 2, LW]) for i in range(4)]
                pg = [ps(ph, f"pg{i}", [128, 2, LW]) for i in range(2)]
                pss = ps(ph, "pss", [128, 16])

                for t, d_, kn in ((cw, convw, "cw"), (cb, convb, "cb"), (lba, ba_d, "lba"), (lbx, bx_d, "lbx"), (cl, lam_d, "cl")):
                    DMA("sync", t[:], d_, "c2" + kn, (), [kn])
                ACT(cl[:], cl[:], AF.Exp, ["cl"], ["cl"], scale=-1.0)
                TS("vector", cl2[:], cl[:], -0.25, 1.0 / 3.0, ALU.mult, ALU.add, ["cl"], ["cl2"])
                TT("vector", cl2[:], cl2[:], cl[:], ALU.mult, ["cl", "cl2"], ["cl2"])
                TS("vector", cl2[:], cl2[:], -0.5, None, ALU.add, None, ["cl2"], ["cl2"])
                TT("vector", cl2[:], cl2[:], cl[:], ALU.mult, ["cl", "cl2"], ["cl2"])
                TS("vector", cl2[:], cl2[:], 1.0, None, ALU.add, None, ["cl2"], ["cl2"])
                TT("vector", cl[:], cl2[:], cl[:], ALU.mult, ["cl", "cl2"], ["cl"])
                TS("vector", cl2[:], cl[:], -16.0, None, ALU.mult, None, ["cl"], ["cl2"])
                TS("vector", cl[:], cl[:], -8.0, None, ALU.mult, None, ["cl", "cl2"], ["cl"])
                for t, d_, kn in ((wbda, wbda_d, "wbda"), (wbdx, wbdx_d, "wbdx")):
                    DMA("sync", stg[0][:, 0:1024], d_.rearrange("p c m -> p (c m)"), "c3", [], ["lstg0"])
                    CP("vector", t[:].rearrange("p c m -> p (c m)"), stg[0][:, 0:1024], ["lstg0"], [kn])
                w_in_v = w_in.rearrange("(k p) n -> p k n", p=128)
                pstg = [t_[:].rearrange("p c t -> p (c t)") for t_ in (xc2[0], xc2[1], gi2[0], gi2[1], ga2[0], ga2[1], gg2[0], gg2[1])]
                for k in range(16):
                    for h_ in range(2):
                        i8 = (2 * k + h_) % 8
                        DMA(hwq(), pstg[i8], w_in_v[:, k, 3072 + h_ * 1024:3072 + (h_ + 1) * 1024], f"lp{i8}", [], [("lpst", i8)])
                        if h_:
                            ACT(wl[:, k, h_ * 1024:(h_ + 1) * 1024], pstg[i8], AF.Copy, [("lpst", i8)], [("wl", k, h_)])
                        else:
                            CP("vector", wl[:, k, h_ * 1024:(h_ + 1) * 1024], pstg[i8], [("lpst", i8)], [("wl", k, h_)])
                S.barrier()
                MS("vector", xr[:], 0.0, [("xr", p_) for p_ in range(4)])
                MS("vector", hh[1][:], 0.0, [("hh1", p_) for p_ in range(4)])
                wl_keys = [("wl", k, h_) for k in range(16) for h_ in range(2)]
                def load_xwin(wi_):
                    DMA(hwq(), stg[wi_ % 2][:], xT[wi_], "lw0", [], ["lstg0"])
                    ACT(xw[wi_ % 2][:].rearrange("p k t -> p (k t)"), stg[wi_ % 2][:], AF.Copy, ["lstg0"], [f"lxw{wi_ % 2}"])

                NWL = 0 if skip_lru else 2 * HALF // LW
                if NWL:
                    load_xwin(0)
                def lru_window(wi):
                    own = wi >= HALF // LW
                    s_ = wi % 2
                    t0 = wi * LW
                    o0 = t0 - HALF
                    wp = wi % 2
                    xc_w, xcb_w, gr_w, gi_w, ga_w, gs_w, gg_w = xc2[wp], xcb2[wp], gr2[wp], gi2[wp], ga2[wp], gs2[wp], gg2[wp]
                    sq_w = xcb_w
                    hcur, hprev = hh[wi % 2], hh[(wi + 1) % 2]
                    hk, hpk = f"hh{wi % 2}", f"hh{(wi + 1) % 2}"
                    for p in range(4):
                        CP("vector", xr[:, 2 * p:2 * p + 2, 0:3], xr[:, 2 * p:2 * p + 2, LW:LW + 3], [("xr", p)], [("xr", p)])
                        for c in (2 * p, 2 * p + 1):
                            MMG(pxr[p][:, c % 2, :], [(wl[:, k, c * 128:(c + 1) * 128], xw[s_][:, k, :]) for k in range(16)],
                                wl_keys + [f"lxw{s_}"], [("pxr", p)])
                        CP("vector", xr[:, 2 * p:2 * p + 2, 3:3 + LW], pxr[p][:, :, :], [("pxr", p)], [("xr", p)])
                    yield
                    if own:
                        for p in range(4):
                            for c in (2 * p, 2 * p + 1):
                                MMG(pxr[p][:, c % 2, :], [(wl[:, k, 1024 + c * 128:1024 + (c + 1) * 128], xw[s_][:, k, :]) for k in range(16)],
                                    wl_keys + [f"lxw{s_}"], [("pxr", p)])
                            ACT(gg_w[:, 2 * p:2 * p + 2, :], pxr[p][:, :, :], AF.Gelu_apprx_tanh, [("pxr", p)], [("gg", wp, p)])
                    yield
                    if wi + 1 < NWL:
                        load_xwin(wi + 1)
                    for p in range(4):
                        for c in (2 * p, 2 * p + 1):
                            TS("vector", xc_w[:, c, :], xr[:, c, 3:3 + LW], cw[:, c, 3:4], cb[:, c:c + 1], ALU.mult, ALU.add,
                               [("xr", p), "cw", "cb"], [("xcc", wp, c)])
                        for j in range(3):
                            for c in (2 * p, 2 * p + 1):
                                STT(xc_w[:, c, :], xr[:, c, j:j + LW], cw[:, c, j:j + 1], xc_w[:, c, :], ALU.mult, ALU.add, [("xr", p), ("xcc", wp, c), "cw"], [("xcc", wp, c)])
                        ACT(xcb_w[:, 2 * p:2 * p + 2, :], xc_w[:, 2 * p:2 * p + 2, :], AF.Copy, [("xcc", wp, 2 * p), ("xcc", wp, 2 * p + 1)], [("xcb", wp, p)])
                    yield
                    for p in range(4):
                        for c in (2 * p, 2 * p + 1):
                            pa = pg[c % 2]
                            MM(pa[:, 0, :], wbda[:, c, :], xcb_w[:, c, :], True, True, ["wbda", ("xcb", wp, p)], [("pg", c % 2)])
                            MM(pa[:, 1, :], wbdx[:, c, :], xcb_w[:, c, :], True, True, ["wbdx", ("xcb", wp, p)], [("pg", c % 2)])
                            ACT(gr_w[:, c, :], pa[:, 0, :], AF.Sigmoid, [("pg", c % 2), "lba"], [("gr", 0, p)], bias=lba[:, c:c + 1])
                            ACT(gi_w[:, c, :], pa[:, 1, :], AF.Sigmoid, [("pg", c % 2), "lbx"], [("gi", wp, p)], bias=lbx[:, c:c + 1])
                    yield
                    for p in range(4):
                        for c in (2 * p, 2 * p + 1):
                            ACT(ga_w[:, c, :], gr_w[:, c, :], AF.Exp, [("gr", 0, p), "cl"], [("ga", wp, p)], scale=cl[:, c:c + 1])
                            ACT(gs_w[:, c, :], gr_w[:, c, :], AF.Exp, [("gr", 0, p), "cl2"], [("gs", 0, p)], scale=cl2[:, c:c + 1])
                    yield
                    for p in range(4):
                        sl2 = slice(2 * p, 2 * p + 2)
                        ACT(gs_w[:, sl2, :], gs_w[:, sl2, :], AF.Sqrt, [("gs", 0, p)], [("gs", 0, p)], bias=1.0, scale=-1.0)
                    yield
                    for p in range(4):
                        sl2 = slice(2 * p, 2 * p + 2)
                        TT("vector", gi_w[:, sl2, :], gi_w[:, sl2, :], xc_w[:, sl2, :], ALU.mult, [("gi", wp, p), ("xcc", wp, 2 * p), ("xcc", wp, 2 * p + 1)], [("gi", wp, p)])
                        if own:
                            TT("vector", gs_w[:, sl2, :], gs_w[:, sl2, :], gi_w[:, sl2, :], ALU.mult, [("gs", 0, p), ("gi", wp, p)], [("gs", 0, p)])
                        else:
                            STT(gs_w[:, sl2, :], gi_w[:, sl2, :], flag[:, 0:1], gs_w[:, sl2, :], ALU.mult, ALU.mult, [("gs", 0, p), ("gi", wp, p), "flag"], [("gs", 0, p)])
                        for c in (2 * p, 2 * p + 1):
                            SCAN(hcur[:, c, :], ga_w[:, c, :], gs_w[:, c, :], hprev[:, c, LW - 1:LW], [("ga", wp, p), ("gs", 0, p), (hpk, p)], [(hk, p)])
                        if own:
                            TT("vector", gg_w[:, sl2, :], gg_w[:, sl2, :], hcur[:, sl2, :], ALU.mult, [("gg", wp, p), (hk, p)], [("gg", wp, p)])
                            ACT(yT[:, 8 + 2 * p:10 + 2 * p, o0:o0 + LW], gg_w[:, sl2, :], AF.Copy, [("gg", wp, p)], [("yT", "l", wi)])
                            TT("vector", sq_w[:, sl2, :], gg_w[:, sl2, :], gg_w[:, sl2, :], ALU.mult, [("gg", wp, p)], [("xcb", wp, p)])
                    if own:
                        for tb in range(LW // 128):
                            tile_i = (o0 + tb * 128) // 128
                            MMG(pss[:, tile_i:tile_i + 1], [(sq_w[:, c, tb * 128:(tb + 1) * 128], onescol_b[:, 0:1]) for c in range(8)],
                                [("xcb", wp, p_) for p_ in range(4)] + ["onescol_b"], ["pss"])

                gens = [lru_window(w_) for w_ in range(NWL)]
                if NWL:
                    for _ in range(3):
                        next(gens[0], None)
                for w_ in range(NWL):
                    nxt = gens[w_ + 1] if w_ + 1 < NWL else None
                    for step in range(4):
                        next(gens[w_], None)
                        if nxt is not None and step < 3:
                            next(nxt, None)
                    for _ in gens[w_]:
                        pass
                if not skip_lru:
                    CP("vector", ssq[:, 16:32], pss[:, 0:16], ["pss"], ["ssq"])
                else:
                    for w_ in range(HALF // LW, 2 * HALF // LW):
                        MS("gpsimd", yT[:, 8:16, (w_ - HALF // LW) * LW:(w_ - HALF // LW + 1) * LW], 0.0, [("yT", "l", w_)])

            if stop_after == "lru":
                pass
            else:
                with contextlib.ExitStack() as ph:
                    ph.callback(S.barrier)
                    accO = sb(ph, "accO", [128, 2, HALF])
                    accD = sb(ph, "accD", [128, 2, HALF])
                    etb = sb(ph, "etb", [128, 3, 4, 256], BF16)
                    qTm = sb(ph, "qTm", [128, 4, HALF], BF16)
                    kT = sb(ph, "kT", [128, 2, 2 * HALF], BF16)
                    vT = sb(ph, "vT", [128, 2, 2 * HALF], BF16)
                    ALV = {"a1": 1, "a2": 2, "a2p0": 2, "a2p1": 2, "a2p2": 2, "a3": 3, "a3p0": 3}.get(stop_after, 9)
                    VPAT = {"a2p0": (0,), "a2p1": (1,), "a2p2": (2,)}.get(stop_after, (0, 1, 2))
                    APAT = PATTERNS[:1] if stop_after == "a3p0" else PATTERNS
                    for pas in range(4 if ALV == 9 else 1):
                        MS("vector", qTm[:], 0.0, ["qTm"])
                        with contextlib.ExitStack() as ip:
                            ip.callback(S.barrier)
                            wq = sb(ip, "wq", [128, 16, 768], BF16)
                            stg = [sb(ip, f"astg{i}", [128, 16 * AW]) for i in range(2)]
                            xw = [sb(ip, f"axw{i}", [128, 16, AW], BF16) for i in range(2)]
                            pq = [ps(ip, f"pq{i}", [128, 2, AW]) for i in range(3)]
                            for pi_ in range(3):
                                s_ = pi_ % 2
                                DMA(hwq(), stg[s_][:, 0:1024].rearrange("p (h q) -> p h q", h=4), etab_d[:, pi_, pas * 4:(pas + 1) * 4, :],
                                    f"aw{s_}", [], [f"astg{s_}"])
                                ACT(etb[:, pi_, :, :], stg[s_][:, 0:1024].rearrange("p (h q) -> p h q", h=4), AF.Copy, [f"astg{s_}"], ["etb"])
                            wq_flat = wq[:].rearrange("p k n -> p (k n)")
                            for i6 in range(6):
                                s_ = i6 % 2
                                DMA(hwq(), stg[s_][:], wqkv_d[pas][:, i6 * 2048:(i6 + 1) * 2048], f"aw{s_}", [], [f"astg{s_}"])
                                if i6 % 2:
                                    ACT(wq_flat[:, i6 * 2048:(i6 + 1) * 2048], stg[s_][:], AF.Copy, [f"astg{s_}"], [("wq", i6)])
                                else:
                                    CP("vector", wq_flat[:, i6 * 2048:(i6 + 1) * 2048], stg[s_][:], [f"astg{s_}"], [("wq", i6)])
                            def load_xwin_a(wi_):
                                q_ = wi_ % 2
                                hf = 8 * AW
                                DMA(hwq(), stg[q_][:], xT[wi_], f"aw{q_}", [], [f"astg{q_}"])
                                ACT(xw[q_][:].rearrange("p k t -> p (k t)")[:, 0:hf], stg[q_][:, 0:hf], AF.Copy, [f"astg{q_}"], [(f"axw{q_}", 0)])
                                ACT(xw[q_][:].rearrange("p k t -> p (k t)")[:, hf:2 * hf], stg[q_][:, hf:2 * hf], AF.Copy, [f"astg{q_}"], [(f"axw{q_}", 1)])

                            load_xwin_a(0)
                            for wi in range(2 * HALF // AW):
                                own = wi >= HALF // AW
                                s_ = wi % 2
                                t0 = wi * AW
                                if wi + 1 < 2 * HALF // AW:
                                    load_xwin_a(wi + 1)
                                for j in ((1, 2, 0) if own else (1, 2)):
                                    pt = pq[j]
                                    for cc in range(2):
                                        col = j * 256 + cc * 128
                                        MMG(pt[:, cc, :], [(wq[:, k, col:col + 128], xw[s_][:, k, :]) for k in range(16)],
                                            [("wq", k) for k in range(6)] + [(f"axw{s_}", 0), (f"axw{s_}", 1)], [("pq", j)])
                                    if j == 1:
                                        CP("vector", kT[:, :, t0:t0 + AW], pt[:, :, :], [("pq", j)], ["kT"])
                                    elif j == 2:
                                        ACT(vT[:, :, t0:t0 + AW], pt[:, :, :], AF.Copy, [("pq", j)], ["vT"])
                                    else:
                                        o0 = t0 - HALF
                                        for hl in range(4):
                                            po = (hl % 2) * 64
                                            ACT(qTm[po:po + 64, hl, o0:o0 + AW], pt[po:po + 64, hl // 2, :], AF.Copy,
                                                [("pq", j)], ["qTm"], scale=0.125)
                        with contextlib.ExitStack() as at:
                            at.callback(S.barrier)
                            NVB = 69
                            vtok = sb(at, "vtok", [128, NVB, 256], BF16)
                            PT = [sb(at, f"PT{i}", [128, 256], BF16) for i in range(6)]
                            EX = [sb(at, f"EX{i}", [128, 256], BF16) for i in range(6)]
                            sqa = sb(at, "sqa", [128, 2, HALF], BF16)
                            vt = contextlib.ExitStack()
                            ptr = [ps(vt, f"ptr{i}", [128, 256], BF16) for i in range(6)]
                            vmap = {}
                            nv = 0
                            for pi, d in enumerate(PATTERNS if ALV >= 2 else ()):
                                if pi not in VPAT:
                                    continue
                                nblk = 32 // d
                                for r_ in range(d):
                                    for b in range(nblk // 2 - 1, nblk):
                                        vmap[(pi, r_, b)] = nv
                                        st_ = d * 128 * b + r_
                                        pt = ptr[nv % 6]
                                        for cc in range(2):
                                            TR(pt[:, cc * 128:(cc + 1) * 128], vT[:, cc, st_:st_ + d * 127 + 1:d], ident_b[:],
                                               ["vT", "ident_b"], [("ptr", nv % 6)])
                                        if nv % 2:
                                            ACT(vtok[:, nv, :], pt[:], AF.Copy, [("ptr", nv % 6)], [("vtok", nv)])
                                        else:
                                            CP("vector", vtok[:, nv, :], pt[:], [("ptr", nv % 6)], [("vtok", nv)])
                                        nv += 1
                            assert nv == NVB or ALV < 9
                            S.barrier()
                            vt.close()
                            pST = [ps(at, f"pST{i}", [128, 256]) for i in range(4)]
                            pO = [ps(at, f"pO{i}", [128, 512]) for i in range(2)]
                            pD = [ps(at, f"pD{i}", [128, 512]) for i in range(2)]
                            items = []
                            gh_ = 0
                            for pi, d in enumerate(APAT if ALV >= 3 else ()):
                                nblk = 32 // d
                                groups = []
                                if d == 1:
                                    for g in range(4):
                                        groups.append(([(0, 16 + 4 * g + u) for u in range(4)],
                                                       lambda a, g=g: a[:, 512 * g:512 * (g + 1)]))
                                elif d == 4:
                                    for c_ in range(4):
                                        groups.append(([(c_, 4 + u) for u in range(4)],
                                                       lambda a, c_=c_: a[:, c_:HALF:4]))
                                else:
                                    for g in range(4):
                                        groups.append(([(4 * g + u, 1) for u in range(4)],
                                                       lambda a, g=g: a.rearrange("p (a r) -> p r a", r=16)[:, 4 * g:4 * g + 4, :]))
                                for units, accview in groups:
                                    for hl in range(4):
                                        for u, (r_, n) in enumerate(units):
                                            items.append((pi, d, nblk, hl, gh_, u, r_, n, accview, u == 3))
                                        gh_ += 1
                            NST, NPT, SKEW = 4, 6, 3

                            def unit_front(i, it):
                                pi, d, nblk, hl, gh, u, r_, n, accview, last = it
                                cc = hl // 2
                                qs = d * 128 * n + r_ - HALF
                                qap = qTm[:, hl, qs:qs + d * 127 + 1:d]
                                sti, pti = i % NST, i % NPT
                                for j, b in enumerate((n - 1, n)):
                                    ks = d * 128 * b + r_
                                    MM(pST[sti][:, j * 128:(j + 1) * 128], kT[:, cc, ks:ks + d * 127 + 1:d], qap, True, True,
                                       ["kT", "qTm"], [("pST", sti)])
                                ACT(EX[pti][:], pST[sti][:], AF.Exp, [("pST", sti)], [("EX", pti)])
                                TT("vector", PT[pti][:], EX[pti][:], etb[:, pi, hl, :], ALU.mult, [("EX", pti), "etb"], [("PT", pti)])

                            pend = []

                            def unit_back(i, it):
                                while pend:
                                    pend.pop(0)()
                                pi, d, nblk, hl, gh, u, r_, n, accview, last = it
                                cc, po = hl // 2, (hl % 2) * 64
                                go, pti = gh % 2, i % NPT
                                for j, b in enumerate((n - 1, n)):
                                    vi = vmap[(pi, r_, b)]
                                    prev_half = b < nblk // 2
                                    MM(pO[go][:, u * 128:(u + 1) * 128], vtok[:, vi, cc * 128:(cc + 1) * 128], PT[pti][:, j * 128:(j + 1) * 128],
                                       j == 0, j == 1, [("vtok", vi), ("PT", pti)], [("pO", go)])
                                    MM(pD[go][:, u * 128:(u + 1) * 128], (pones_b if prev_half else ones_b)[:], PT[pti][:, j * 128:(j + 1) * 128],
                                       j == 0, j == 1, ["pones_b", "ones_b", ("PT", pti)], [("pD", go)])
                                if not last:
                                    return
                                ao = accview(accO[po:po + 64, cc, :])
                                ad = accview(accD[po:po + 64, cc, :])
                                so = pO[go][po:po + 64, :]
                                sd = pD[go][po:po + 64, :]
                                if d == 16:
                                    so = so.rearrange("p (u q) -> p u q", u=4)
                                    sd = sd.rearrange("p (u q) -> p u q", u=4)
                                if pi == 0:
                                    ACT(ao, so, AF.Copy, [("pO", go)], [("accO", hl)])
                                    pend.append(lambda: ACT(ad, sd, AF.Copy, [("pD", go)], [("accD", hl)]))
                                else:
                                    TT("vector", ao, so, ao, ALU.add, [("pO", go), ("accO", hl)], [("accO", hl)])
                                    pend.append(lambda: TT("vector", ad, sd, ad, ALU.add, [("pD", go), ("accD", hl)], [("accD", hl)]))

                            for i in range(len(items) + SKEW if items else 0):
                                if i < len(items):
                                    unit_front(i, items[i])
                                if i >= SKEW:
                                    unit_back(i - SKEW, items[i - SKEW])
                            while pend:
                                pend.pop(0)()
                            acck = [("accO", h_) for h_ in range(4)] + [("accD", h_) for h_ in range(4)]
                            for cc in range(2 if ALV >= 9 else 0):
                                REC(accD[:, cc, :], accD[:, cc, :], acck, acck)
                                TT("vector", accO[:, cc, :], accO[:, cc, :], accD[:, cc, :], ALU.mult, acck, acck)
                                ACT(yT[:, pas * 2 + cc, :], accO[:, cc, :], AF.Copy, acck, [("yT", "a", pas)])
                                TT("vector", sqa[:, cc, :], accO[:, cc, :], accO[:, cc, :], ALU.mult, acck, ["sqa"])
                            if ALV >= 9:
                                S.barrier()
                                psa = pST[0]
                            for ti in range(16 if ALV >= 9 else 0):
                                MMG(psa[:, ti:ti + 1], [(sqa[:, cc, ti * 128:(ti + 1) * 128], onescol_b[:, 0:1]) for cc in range(2)],
                                    ["sqa", "onescol_b"], ["psa"])
                            if ALV >= 9:
                                TT("vector", ssq[:, 0:16], psa[:, 0:16], ssq[:, 0:16], ALU.add, ["psa", "ssq"], ["ssq"])

            if debug:
                with contextlib.ExitStack() as dd:
                    dd.callback(S.barrier)
                    dtmp = sb(dd, "dtmp", [128, 16, HALF])
                    CP("vector", dtmp[:], yT[:], [("yT", "l", w_) for w_ in range(HALF // LW, 2 * HALF // LW)] + [("yT", "a", p_) for p_ in range(4)], ["dtmp"])
                    DMA("sync", dbg["yT"], dtmp[:], "dbg", ["dtmp"], [], final=True)
                    DMA("sync", dbg["ssq"], ssq[:], "dbg", ["ssq"], [], final=True)

            yT_keys = [("yT", "l", w_) for w_ in range(HALF // LW, 2 * HALF // LW)] + [("yT", "a", p_) for p_ in range(4)]

            if stop_after in ("lru", "mixer", "a1", "a2", "a2p0", "a2p1", "a2p2", "a3", "a3p0"):
                pass
            else:
                with contextlib.ExitStack() as ph:
                    ph.callback(S.barrier)
                    wo = sb(ph, "wo", [128, 16, D], BF16)
                    gcat = sb(ph, "gcat", [128, 16])
                    g1 = sb(ph, "g1", [128, D])
                    b1 = sb(ph, "b1", [128, D])
                    wr = sb(ph, "wr", [128, 16, 36])
                    brt = sb(ph, "brt", [128, 36])
                    tri = sb(ph, "tri", [128, 128])
                    ebase = sb(ph, "ebase", [128, NE])
                    cm = sb(ph, "cm", [128, NE])
                    rstd = sb(ph, "rstd", [128, 32])
                    DMA("sync", gcat[:], gcat_d, "k3", [], ["gcat"])
                    DMA("sync", g1[:], ln1g_d, "k4", [], ["g1"])
                    DMA("sync", b1[:], ln1b_d, "k5", [], ["b1"])
                    DMA("sync", wr[:], wr_d.rearrange("(k p) n -> p k n", p=128), "k6", [], ["wr"])
                    DMA("sync", brt[:], br_d, "k7", [], ["brt"])
                    DMA("sync", tri[:], tri_d, "k8", [], ["tri"])
                    DMA("sync", ebase[:], ebase_d, "k9", [], ["ebase"])
                    MS("vector", cm[:], 0.0, ["cm"])
                    TS("vector", rstd[:], ssq[:], 1.0 / 1024.0, RMS_EPS, ALU.mult, ALU.add, ["ssq"], ["rstd"])
                    ACT(rstd[:], rstd[:], AF.Sqrt, ["rstd"], ["rstd"])
                    REC(rstd[:], rstd[:], ["rstd"], ["rstd"])
                    with contextlib.ExitStack() as wp:
                        wp.callback(S.barrier)
                        stg = [sb(wp, f"ostg{i}", [128, D]) for i in range(2)]
                        w_out_v = w_out.rearrange("(k p) n -> p k n", p=128)
                        for k in range(16):
                            s_ = k % 2
                            DMA(hwq(), stg[s_][:], w_out_v[:, k, :], f"ow{s_}", [], [f"ostg{s_}"])
                            if k % 2:
                                ACT(wo[:, k, :], stg[s_][:], AF.Copy, [f"ostg{s_}", "gcat"], [("wo", k)], scale=gcat[:, k:k + 1])
                            else:
                                TS("vector", wo[:, k, :], stg[s_][:], gcat[:, k:k + 1], None, ALU.mult, None,
                                   [f"ostg{s_}", "gcat"], [("wo", k)])
                    with contextlib.ExitStack() as tl:
                        tl.callback(S.barrier)
                        zb = [sb(tl, f"zb{i}", [128, D]) for i in range(2)]
                        x1 = sb(tl, "x1", [128, D])
                        x1b = sb(tl, "x1b", [128, D], BF16)
                        x1T = sb(tl, "x1T", [128, 16, 128])
                        st6s = [sb(tl, f"st6{i}", [128, 4, 6]) for i in range(2)]
                        mv = sb(tl, "mv", [128, 2])
                        sm = sb(tl, "sm", [128, 16])
                        lg = sb(tl, "lg", [128, 36])
                        lm = sb(tl, "lm", [128, NE])
                        lm2 = sb(tl, "lm2", [128, NE])
                        mk1 = sb(tl, "mk1", [128, NE])
                        mk2 = sb(tl, "mk2", [128, NE])
                        mk = sb(tl, "mk", [128, NE])
                        slot = sb(tl, "slot", [128, NE])
                        ovf = sb(tl, "ovf", [128, NE])
                        g4 = sb(tl, "g4", [128, 4])
                        oh4 = sb(tl, "oh4", [128, 4])
                        slf = sb(tl, "slf", [128, 2])
                        pA = [ps(tl, f"pA{i}", [128, 512]) for i in range(2)]
                        pL = [ps(tl, f"pL{i}", [128, 512]) for i in range(2)]
                        pT = [ps(tl, f"pT{i}", [128, 512]) for i in range(2)]
                        pR = ps(tl, "pR", [128, 36])
                        pC = ps(tl, "pC", [128, NE])
                        wo_keys = [("wo", k) for k in range(16)]
                        def part_a(ti):
                            z = zb[ti % 2]
                            zk = f"zb{ti % 2}"
                            tk0 = ti * 128
                            st6 = st6s[ti % 2]
                            s6k = f"st6{ti % 2}"
                            DMA(hwq(), z[:], xtok[tk0:tk0 + 128, :], f"xt{ti % 2}", [], [zk])
                            ACT(z[:], z[:], AF.Copy, [zk], [zk], scale=ALPHA)
                            for nb_ in range(4):
                                b_ = (ti * 4 + nb_) % 2
                                MMG(pA[b_][:], [(yT[:, k, tk0:tk0 + 128], wo[:, k, nb_ * 512:(nb_ + 1) * 512]) for k in range(8)],
                                    yT_keys + wo_keys, [("pA", b_)])
                                MMG(pL[b_][:], [(yT[:, k, tk0:tk0 + 128], wo[:, k, nb_ * 512:(nb_ + 1) * 512]) for k in range(8, 16)],
                                    yT_keys + wo_keys, [("pL", b_)])
                                zs = z[:, nb_ * 512:(nb_ + 1) * 512]
                                STT(zs, pA[b_][:], rstd[:, ti:ti + 1], zs, ALU.mult, ALU.add, [("pA", b_), "rstd", zk], [zk])
                                STT(zs, pL[b_][:], rstd[:, 16 + ti:17 + ti], zs, ALU.mult, ALU.add, [("pL", b_), "rstd", zk], [zk])
                                BNS(st6[:, nb_, :], zs, [zk], [s6k])
                                yield
                        def part_b(ti):
                            z = zb[ti % 2]
                            zk = f"zb{ti % 2}"
                            tk0 = ti * 128
                            st6 = st6s[ti % 2]
                            s6k = f"st6{ti % 2}"
                            BNA(mv[:], st6[:].rearrange("p a b -> p (a b)"), [s6k], ["mv"])
                            TS("vector", sm[:, 0:1], mv[:, 1:2], LN_EPS, None, ALU.add, None, ["mv"], ["sm"])
                            ACT(sm[:, 0:1], sm[:, 0:1], AF.Sqrt, ["sm"], ["sm"])
                            REC(sm[:, 0:1], sm[:, 0:1], ["sm"], ["sm"])
                            STT(sm[:, 1:2], mv[:, 0:1], -1.0, sm[:, 0:1], ALU.mult, ALU.mult, ["mv", "sm"], ["sm"])
                            ACT(x1[:], z[:], AF.Identity, [zk, "sm"], ["x1"], bias=sm[:, 1:2], scale=sm[:, 0:1])
                            TT("vector", x1[:], x1[:], g1[:], ALU.mult, ["x1", "g1"], ["x1"])
                            TT("vector", x1[:], x1[:], b1[:], ALU.add, ["x1", "b1"], ["x1"])
                            DMA("sync", x1s[tk0:tk0 + 128, :], x1[:], "x1st", ["x1"], [("x1s", ti)])
                            if debug:
                                DMA("sync", dbg["x1"][tk0:tk0 + 128, :], x1[:], "dbg", ["x1"], [], final=True)
                            ACT(x1b[:], x1[:], AF.Copy, ["x1"], ["x1b"])
                            yield
                            for k4 in range(4):
                                pt = pT[k4 % 2]
                                for kk in range(4):
                                    k = k4 * 4 + kk
                                    TR(pt[:, kk * 128:(kk + 1) * 128], x1[:, k * 128:(k + 1) * 128], ident_f[:], ["x1", "ident_f"], [("pT", k4 % 2)])
                                if k4 % 2:
                                    ACT(x1T[:, k4 * 4:(k4 + 1) * 4, :], pt[:].rearrange("p (a b) -> p a b", a=4), AF.Copy, [("pT", k4 % 2)], ["x1T"])
                                else:
                                    CP("vector", x1T[:, k4 * 4:(k4 + 1) * 4, :], pt[:].rearrange("p (a b) -> p a b", a=4), [("pT", k4 % 2)], ["x1T"])
                            MMG(pR[:], [(x1T[:, k, :], wr[:, k, :]) for k in range(16)], ["x1T", "wr"], ["pR"])
                            TT("vector", lg[:], pR[:], brt[:], ALU.add, ["pR", "brt"], ["lg"])
                            yield
                            RED(sm[:, 2:3], lg[:, 0:4], ALU.max, ["lg"], ["sm"])
                            TS("vector", sm[:, 3:4], sm[:, 2:3], -1.0, None, ALU.mult, None, ["sm"], ["sm"])
                            ACT(g4[:], lg[:, 0:4], AF.Exp, ["lg", "sm"], ["g4", "sm"], bias=sm[:, 3:4], accum=sm[:, 4:5])
                            REC(sm[:, 5:6], sm[:, 4:5], ["sm"], ["sm"])
                            TS("vector", oh4[:], lg[:, 0:4], sm[:, 2:3], None, ALU.is_equal, None, ["lg", "sm"], ["oh4"])
                            TS("vector", oh4[:], oh4[:], -1.0, 1.0e30, ALU.add, ALU.mult, ["oh4"], ["oh4"])
                            for g in range(4):
                                TS("vector", lm[:, 8 * g:8 * g + 8], lg[:, 4 + 8 * g:12 + 8 * g], oh4[:, g:g + 1], None, ALU.add, None,
                                   ["lg", "oh4"], ["lm"])
                            RED(sm[:, 6:7], lm[:], ALU.max, ["lm"], ["sm"])
                            TS("vector", mk1[:], lm[:], sm[:, 6:7], None, ALU.is_equal, None, ["lm", "sm"], ["mk1"])
                            STT(lm2[:], mk1[:], -1.0e30, lm[:], ALU.mult, ALU.add, ["mk1", "lm"], ["lm2"])
                            RED(sm[:, 7:8], lm2[:], ALU.max, ["lm2"], ["sm"])
                            TS("vector", mk2[:], lm2[:], sm[:, 7:8], None, ALU.is_equal, None, ["lm2", "sm"], ["mk2"])
                            TT("vector", sm[:, 8:9], sm[:, 6:7], sm[:, 7:8], ALU.subtract, ["sm"], ["sm"])
                            ACT(sm[:, 9:10], sm[:, 8:9], AF.Sigmoid, ["sm"], ["sm"])
                            ACT(sm[:, 10:11], sm[:, 8:9], AF.Sigmoid, ["sm"], ["sm"], scale=-1.0)
                            TT("vector", wts[:, ti, 0:1], sm[:, 9:10], sm[:, 5:6], ALU.mult, ["sm"], ["wts"])
                            TT("vector", wts[:, ti, 1:2], sm[:, 10:11], sm[:, 5:6], ALU.mult, ["sm"], ["wts"])
                            TT("vector", mk[:], mk1[:], mk2[:], ALU.add, ["mk1", "mk2"], ["mk"])
                            yield
                            MM(pC[:], tri[:], mk[:], True, False, ["tri", "mk"], ["pC"])
                            MM(pC[:], ones_f[:], cm[:], False, True, ["ones_f", "cm"], ["pC"])
                            TS("vector", ovf[:], pC[:], CAP - 0.5, BIG, ALU.is_ge, ALU.mult, ["pC"], ["ovf"])
                            TT("vector", slot[:], pC[:], ebase[:], ALU.add, ["pC", "ebase"], ["slot"])
                            TT("vector", slot[:], slot[:], ovf[:], ALU.add, ["slot", "ovf"], ["slot"])
                            TT("vector", cm[:], cm[:], mk[:], ALU.add, ["cm", "mk"], ["cm"])
                            TT("vector", mk1[:], mk1[:], slot[:], ALU.mult, ["mk1", "slot"], ["mk1"])
                            TT("vector", mk2[:], mk2[:], slot[:], ALU.mult, ["mk2", "slot"], ["mk2"])
                            RED(slf[:, 0:1], mk1[:], ALU.add, ["mk1"], ["slf"])
                            RED(slf[:, 1:2], mk2[:], ALU.add, ["mk2"], ["slf"])
                            CP("vector", slots_i[:, ti, :], slf[:], ["slf"], [("slots", ti)])
                            if debug:
                                CP("vector", sm[:, 12:14], slf[:], ["slf"], ["sm"])
                                CP("vector", sm[:, 14:16], wts[:, ti, :], ["wts", "sm"], ["sm"])
                                DMA("sync", dbg["rt"][:, ti, :], sm[:, 12:16], "dbg", ["sm"], [], final=True)
                            for a_ in range(2):
                                SCAT(xs_d, slots_i[:, ti, a_:a_ + 1], x1b[:], "scat", ["x1b", ("slots", ti)], [("xs", ti, a_)])

                        for _ in part_a(0):
                            pass
                        for ti in range(1, 16):
                            gb = part_b(ti - 1)
                            for _ in part_a(ti):
                                next(gb, None)
                            for _ in gb:
                                pass
                        for _ in part_b(15):
                            pass

        if stop_after in ("lru", "mixer", "ln1", "a1", "a2", "a2p0", "a2p1", "a2p2", "a3", "a3p0"):
            with contextlib.ExitStack() as dd:
                dd.callback(S.barrier)
                zt = sb(dd, "zt", [128, D])
                MS("vector", zt[:], 0.0, ["zt"])
                for ti in range(16):
                    DMA("sync", out_d[ti * 128:(ti + 1) * 128, :], zt[:], "outst", ["zt"], [], final=True)
        else:
            with contextlib.ExitStack() as ph:
                ph.callback(S.barrier)
                w1b = [sb(ph, f"w1b{i}", [128, 16, DE], BF16) for i in range(2)]
                w3b = [sb(ph, f"w3b{i}", [128, 16, DE], BF16) for i in range(2)]
                w2b = [sb(ph, f"w2b{i}", [128, 4, D], BF16) for i in range(2)]
                stg = [sb(ph, f"estg{i}", [128, 2048]) for i in range(NSTG)]
                xe = sb(ph, "xe", [128, 2, D], BF16)
                xeT = sb(ph, "xeT", [128, 16, CAP], BF16)
                h1 = sb(ph, "h1", [128, CAP])
                hT = sb(ph, "hT", [128, 4, CAP], BF16)
                yo = [sb(ph, f"yo{i}", [128, D]) for i in range(2)]
                ptr = [ps(ph, f"etr{i}", [128, 512], BF16) for i in range(2)]
                ph1 = [ps(ph, f"ph1{i}", [128, CAP]) for i in range(2)]
                ph3 = [ps(ph, f"ph3{i}", [128, CAP]) for i in range(2)]
                py = [ps(ph, f"py{i}", [128, 512]) for i in range(2)]
                sctr = [0]
                cast_engs = ["vector", "scalar", "vector", "gpsimd", "scalar", "vector", "scalar", "vector", "gpsimd", "vector", "scalar", "gpsimd"]

                def load_expert(e_):
                    s_ = e_ % 2
                    for wt, src, nk in ((w1b[s_], w1_d[e_], 16), (w3b[s_], w3_d[e_], 16)):
                        for k4 in range(4):
                            si = sctr[0] % NSTG
                            sctr[0] += 1
                            DMA(hwq(), stg[si][:], src[:, k4 * 4 * DE:(k4 + 1) * 4 * DE], f"ew{si}", [], [f"estg{si}"])
                            ce = cast_engs[sctr[0] % 12]
                            dst = wt[:, k4 * 4:(k4 + 1) * 4, :].rearrange("p k n -> p (k n)")
                            if ce == "scalar":
                                ACT(dst, stg[si][:], AF.Copy, [f"estg{si}"], [(wt.name, k4)])
                            else:
                                CP(ce, dst, stg[si][:], [f"estg{si}"], [(wt.name, k4)])
                            yield
                    src = w2_d[e_].rearrange("(k p) n -> p k n", p=128)
                    for k in range(4):
                        si = sctr[0] % NSTG
                        sctr[0] += 1
                        DMA(hwq(), stg[si][:], src[:, k, :], f"ew{si}", [], [f"estg{si}"])
                        ce = cast_engs[sctr[0] % 12]
                        if ce == "scalar":
                            ACT(w2b[s_][:, k, :], stg[si][:], AF.Copy, [f"estg{si}"], [(w2b[s_].name, k)])
                        else:
                            CP(ce, w2b[s_][:, k, :], stg[si][:], [f"estg{si}"], [(w2b[s_].name, k)])
                        yield

                ldr = [iter(())]

                def pump(n):
                    for _ in range(n):
                        next(ldr[0], None)

                for _ in load_expert(0):
                    pass
                for e_ in range(NE):
                    s_ = e_ % 2
                    ldr[0] = load_expert(e_ + 1) if e_ + 1 < NE else iter(())
                    r0 = e_ * CAP
                    DMA("sync", xe[:], xs_d[r0:r0 + CAP, :].rearrange("(b p) d -> p b d", p=128), "xe", [("xs", t_, a2) for t_ in range(16) for a2 in range(2)], ["xe"])
                    pump(2)
                    for b in range(2):
                        for k4 in range(4):
                            pt = ptr[(b * 4 + k4) % 2]
                            pk = ("etr", (b * 4 + k4) % 2)
                            for kk in range(4):
                                k = k4 * 4 + kk
                                TR(pt[:, kk * 128:(kk + 1) * 128], xe[:, b, k * 128:(k + 1) * 128], ident_b[:], ["xe", "ident_b"], [pk])
                            dst = xeT[:, k4 * 4:(k4 + 1) * 4, b * 128:(b + 1) * 128]
                            srcp = pt[:].rearrange("p (a q) -> p a q", a=4)
                            if k4 % 2:
                                ACT(dst, srcp, AF.Copy, [pk], ["xeT"])
                            else:
                                CP("vector", dst, srcp, [pk], ["xeT"])
                            pump(1)
                    w1k = [(w1b[s_].name, k4) for k4 in range(4)]
                    w3k = [(w3b[s_].name, k4) for k4 in range(4)]
                    w2k = [(w2b[s_].name, k) for k in range(4)]
                    for m in range(4):
                        b_ = m % 2
                        MMG(ph1[b_][:], [(w1b[s_][:, k, m * 128:(m + 1) * 128], xeT[:, k, :]) for k in range(16)],
                            w1k + ["xeT"], [("ph1", b_)])
                        MMG(ph3[b_][:], [(w3b[s_][:, k, m * 128:(m + 1) * 128], xeT[:, k, :]) for k in range(16)],
                            w3k + ["xeT"], [("ph3", b_)])
                        ACT(h1[:], ph1[b_][:], AF.Silu, [("ph1", b_)], ["h1"])
                        TT("vector", hT[:, m, :], h1[:], ph3[b_][:], ALU.mult, ["h1", ("ph3", b_)], ["hT"])
                        pump(2)
                    for b in range(2):
                        yb_ = yo[b]
                        for n_ in range(4):
                            pb = (b * 4 + n_) % 2
                            MMG(py[pb][:], [(hT[:, m, b * 128:(b + 1) * 128], w2b[s_][:, m, n_ * 512:(n_ + 1) * 512]) for m in range(4)],
                                w2k + ["hT"], [("py", pb)])
                            if n_ % 2:
                                ACT(yb_[:, n_ * 512:(n_ + 1) * 512], py[pb][:], AF.Copy, [("py", pb)], [yb_.name])
                            else:
                                CP("vector", yb_[:, n_ * 512:(n_ + 1) * 512], py[pb][:], [("py", pb)], [yb_.name])
                            pump(1)
                        DMA("scalar", ys_d[r0 + b * 128:r0 + (b + 1) * 128, :], yb_[:], "yst", [yb_.name], [("ys", e_, b)])
                    pump(99)

            with contextlib.ExitStack() as ph:
                ph.callback(S.barrier)
                g2 = sb(ph, "g2", [128, D])
                b2 = sb(ph, "b2", [128, D])
                ya = [sb(ph, f"ya{i}", [128, D]) for i in range(2)]
                yb = [sb(ph, f"yb{i}", [128, D]) for i in range(2)]
                xz = [sb(ph, f"xz{i}", [128, D]) for i in range(2)]
                oo = [sb(ph, f"oo{i}", [128, D]) for i in range(2)]
                st6 = sb(ph, "st6b", [128, 4, 6])
                mv = sb(ph, "mvb", [128, 2])
                sm = sb(ph, "smb", [128, 4])
                x1s_keys = [("x1s", t_) for t_ in range(16)]
                ys_keys = [("ys", e2, b2_) for e2 in range(NE) for b2_ in range(2)]
                DMA("sync", g2[:], ln2g_d, "k10", [], ["g2"])
                DMA("sync", b2[:], ln2b_d, "k11", [], ["b2"])
                for ti in range(16):
                    i_ = ti % 2
                    tk0 = ti * 128
                    MS("vector", ya[i_][:], 0.0, [f"ya{i_}"])
                    MS("vector", yb[i_][:], 0.0, [f"yb{i_}"])
                    for a_, dst, dk in ((0, ya[i_], f"ya{i_}"), (1, yb[i_], f"yb{i_}")):
                        GATH(dst[:], ys_d, slots_i[:, ti, a_:a_ + 1], f"gath{i_}{a_}", ys_keys + [("slots", ti)], [dk])
                    DMA(hwq(), xz[i_][:], x1s[tk0:tk0 + 128, :], f"xz{i_}", x1s_keys, [f"xz{i_}"])
                    ACT(xz[i_][:], xz[i_][:], AF.Copy, [f"xz{i_}"], [f"xz{i_}"], scale=ALPHA)
                    STT(xz[i_][:], ya[i_][:], wts[:, ti, 0:1], xz[i_][:], ALU.mult, ALU.add, [f"ya{i_}", "wts", f"xz{i_}"], [f"xz{i_}"])
                    STT(xz[i_][:], yb[i_][:], wts[:, ti, 1:2], xz[i_][:], ALU.mult, ALU.add, [f"yb{i_}", "wts", f"xz{i_}"], [f"xz{i_}"])
                    for nb_ in range(4):
                        BNS(st6[:, nb_, :], xz[i_][:, nb_ * 512:(nb_ + 1) * 512], [f"xz{i_}"], ["st6b"])
                    BNA(mv[:], st6[:].rearrange("p a b -> p (a b)"), ["st6b"], ["mvb"])
                    TS("vector", sm[:, 0:1], mv[:, 1:2], LN_EPS, None, ALU.add, None, ["mvb"], ["smb"])
                    ACT(sm[:, 0:1], sm[:, 0:1], AF.Sqrt, ["smb"], ["smb"])
                    REC(sm[:, 0:1], sm[:, 0:1], ["smb"], ["smb"])
                    STT(sm[:, 1:2], mv[:, 0:1], -1.0, sm[:, 0:1], ALU.mult, ALU.mult, ["mvb", "smb"], ["smb"])
                    ACT(oo[i_][:], xz[i_][:], AF.Identity, [f"xz{i_}", "smb"], [f"oo{i_}"], bias=sm[:, 1:2], scale=sm[:, 0:1])
                    TT("vector", oo[i_][:], oo[i_][:], g2[:], ALU.mult, [f"oo{i_}", "g2"], [f"oo{i_}"])
                    TT("vector", oo[i_][:], oo[i_][:], b2[:], ALU.add, [f"oo{i_}", "b2"], [f"oo{i_}"])
                    DMA("sync", out_d[tk0:tk0 + 128, :], oo[i_][:], "outst", [f"oo{i_}"], [], final=True)

        S.emit()
    return nc


def _host_consts():
    slopes = np.exp2(-8.0 * np.arange(1, NH + 1, dtype=np.float64) / NH)
    i = np.arange(128)[:, None]
    q = np.arange(128)[None, :]
    etab = np.zeros((128, 3, NH, 256), np.float32)
    for pi, d in enumerate(PATTERNS):
        for h in range(NH):
            dist0 = q + 128 - i
            dist1 = q - i
            etab[:, pi, h, 0:128] = np.where(dist0 <= 128, np.exp(-slopes[h] * d * np.minimum(dist0, 128)), 0.0)
            etab[:, pi, h, 128:256] = np.where(dist1 >= 0, np.exp(-slopes[h] * d * np.maximum(dist1, 0)), 0.0)
    ident = np.eye(128, dtype=np.float32)
    tri = (np.arange(128)[:, None] < np.arange(128)[None, :]).astype(np.float32)
    ebase = np.tile((np.arange(NE, dtype=np.float32) * CAP)[None, :], (128, 1))
    return etab, ident, tri, ebase


def _chunked(v):
    return np.ascontiguousarray(np.asarray(v, np.float32).reshape(8, 128).T)


def make_in_maps(x, w_in, conv_w, conv_b, lru_wa, lru_ba, lru_wx, lru_bx, lru_lambda, attn_norm_g, lru_norm_g,
                 w_out, ln1_g, ln1_b, router_grp_w, router_grp_b, router_exp_w, router_exp_b, w1, w3, w2,
                 ln2_g, ln2_b):
    f = lambda a: np.ascontiguousarray(np.asarray(a, np.float32))
    x = f(x)
    etab, ident, tri, ebase = _host_consts()
    convw = np.ascontiguousarray(f(conv_w)[0].reshape(4, 8, 128).transpose(2, 1, 0))

    def bd(wg):
        wg = f(wg)[0]
        o = np.zeros((128, 8, 128), np.float32)
        for n in range(16):
            c, hlf = n // 2, n % 2
            o[hlf * 64:(hlf + 1) * 64, c, hlf * 64:(hlf + 1) * 64] = wg[n]
        return o
    gcat = np.concatenate([_chunked(f(attn_norm_g)[0]), _chunked(f(lru_norm_g)[0])], axis=1)
    bc = lambda v: np.ascontiguousarray(np.broadcast_to(f(v).reshape(1, -1), (128, f(v).size)))
    wr = np.ascontiguousarray(np.concatenate([f(router_grp_w)[0], f(router_exp_w)[0]], axis=1))
    br = bc(np.concatenate([f(router_grp_b)[0], f(router_exp_b)[0]]))
    pk = lambda w: np.ascontiguousarray(f(w)[0].reshape(NE, 16, 128, DE).transpose(0, 2, 1, 3)).reshape(NE, 128, 16 * DE)
    wi_ = f(w_in)[0]
    wqkv = np.stack([np.concatenate([wi_[:, j * 1024 + pas * 256:j * 1024 + (pas + 1) * 256] for j in range(3)], axis=1) for pas in range(4)])
    wqkv = np.ascontiguousarray(wqkv.reshape(4, 16, 128, 768).transpose(0, 2, 1, 3)).reshape(4, 128, 16 * 768)
    shared = dict(
        w_in=wi_, w_out=f(w_out)[0], convw=convw, convb=_chunked(f(conv_b)[0]),
        lba=_chunked(f(lru_ba)[0].reshape(-1)), lbx=_chunked(f(lru_bx)[0].reshape(-1)), lam=_chunked(f(lru_lambda)[0]),
        wbda=bd(lru_wa), wbdx=bd(lru_wx), gcat=gcat,
        ln1g=bc(f(ln1_g)[0]), ln1b=bc(f(ln1_b)[0]), ln2g=bc(f(ln2_g)[0]), ln2b=bc(f(ln2_b)[0]),
        wr=wr, br=br, w1=pk(w1), w3=pk(w3), w2=f(w2)[0], wqkv=wqkv,
        etab=etab, ident=ident, tri=tri, ebase=ebase,
    )
    maps = []
    for c in range(8):
        b, h = c // 2, c % 2
        own = x[b, h * HALF:(h + 1) * HALF]
        prev = x[b, 0:HALF] if h == 1 else np.zeros((HALF, D), np.float32)
        xfull = np.concatenate([prev, own], axis=0)
        xT = np.ascontiguousarray(xfull.reshape(2 * HALF // LW, LW, 16, 128).transpose(0, 3, 2, 1)).reshape(2 * HALF // LW, 128, 16 * LW)
        m = dict(shared)
        m["xT"] = xT
        m["xtok"] = np.ascontiguousarray(own)
        m["flag"] = np.full((128, 1), float(h), np.float32)
        maps.append(m)
    return maps


def kernel(**inputs):
    nc = build_program()
    maps = make_in_maps(**inputs)
    res = run_bass_kernel_spmd(nc, maps, core_ids=list(range(8)))
    out = np.zeros((NB, SEQ, D), np.float32)
    for c in range(8):
        b, h = c // 2, c % 2
        out[b, h * HALF:(h + 1) * HALF] = np.asarray(res.results[c]["out"], np.float32)
    return out
```

```python
import contextlib
import numpy as np
import ml_dtypes
import concourse.bass as bass
import concourse.mybir as mybir
from concourse.alu_op_type import AluOpType as ALU
from concourse.bass_utils import run_bass_kernel_spmd

F32 = mybir.dt.float32
BF16 = mybir.dt.bfloat16
I32 = mybir.dt.int32
AF = mybir.ActivationFunctionType
AX = mybir.AxisListType

D = 2048
SEQ = 4096
NB = 4
HALF = 2048
NH = 16
DLRU = 1024
DPROJ = 5120
NE = 32
DE = 512
CAP = 256
ALPHA = 2.0 ** 0.25
LN_EPS = 1e-5
RMS_EPS = 1e-6
PATTERNS = (1, 4, 16)
WIN = 256
LW = 128
AW = 128
BIG = 1.0e6
NSTG = 8

ENGS = ("sync", "scalar", "vector", "gpsimd", "tensor")


class Sched:
    def __init__(self, nc, self_sync=True):
        self.nc = nc
        self.self_sync = self_sync
        self.q = {e: [] for e in ENGS}
        self.cnt = {}
        self.waited = {e: {} for e in ENGS}
        self.last_w = {}
        self.readers = {}
        self.sems = {}
        self.semkeys = []
        self.final = []
        self.bar = {}

    def barrier(self):
        self.bar = dict(self.cnt)

    def _semkey(self, k):
        if k not in self.cnt:
            self.cnt[k] = 0
            self.semkeys.append(k)
        return k

    def _deps(self, eng, r, w):
        deps = {}

        def add(t):
            if t is None:
                return
            k, v = t
            if deps.get(k, 0) < v:
                deps[k] = v
        for key in r:
            add(self.last_w.get(key))
        for key in w:
            add(self.last_w.get(key))
            for t in self.readers.get(key, ()):
                add(t)
        for k, v in self.bar.items():
            if v:
                add((k, v))
        out = []
        for k, v in deps.items():
            if k == eng and (eng != "vector" or not self.self_sync):
                continue
            if self.waited[eng].get(k, 0) >= v:
                continue
            self.waited[eng][k] = v
            out.append((k, v))
        return out

    def _record(self, ticket, r, w):
        for key in w:
            self.last_w[key] = ticket
            self.readers[key] = []
        for key in r:
            self.readers.setdefault(key, []).append(ticket)

    def op(self, eng, fn, r=(), w=()):
        waits = self._deps(eng, r, w)
        k = self._semkey(eng)
        self.cnt[k] += 1
        ticket = (k, self.cnt[k])
        self.q[eng].append((fn, waits, k, 1))
        self._record(ticket, r, w)
        return ticket

    def dma(self, eng, fn, sem, r=(), w=(), final=False):
        waits = self._deps(eng, r, w)
        k = self._semkey("dma:" + sem)
        self.cnt[k] += 16
        ticket = (k, self.cnt[k])
        self.q[eng].append((fn, waits, k, 16))
        self._record(ticket, r, w)
        if final:
            self.final.append(ticket)
        return ticket

    def emit(self):
        nc = self.nc
        with contextlib.ExitStack() as st:
            for k in self.semkeys:
                self.sems[k] = st.enter_context(nc.semaphore(k.replace(":", "_")))
            fin = {}
            for k, v in self.final:
                fin[k] = max(fin.get(k, 0), v)
            block = st.enter_context(nc.Block())
            for e in ENGS:
                ops = self.q[e]
                extra = fin if e == "sync" else {}
                if not ops and not extra:
                    continue

                def body(eh, ops=ops, extra=extra):
                    for fn, waits, k, inc in ops:
                        for wk, wv in waits:
                            eh.wait_ge(self.sems[wk], wv)
                        fn(eh).then_inc(self.sems[k], inc)
                    for wk, wv in extra.items():
                        eh.wait_ge(self.sems[wk], wv)

                getattr(block, e)(body)


def build_program(stop_after=None, debug=False, skip_lru=False):
    nc = bass.Bass("TRN2", target_bir_lowering=False)

    def din(name, shape, dt=F32):
        return nc.dram_tensor(name, list(shape), dt, kind="ExternalInput").ap()

    xT = din("xT", [2 * HALF // LW, 128, 16 * LW])
    xtok = din("xtok", [HALF, D])
    w_in = din("w_in", [D, DPROJ])
    w_out = din("w_out", [D, D])
    wqkv_d = din("wqkv", [4, 128, 16 * 768])
    convw = din("convw", [128, 8, 4])
    convb = din("convb", [128, 8])
    ba_d = din("lba", [128, 8])
    bx_d = din("lbx", [128, 8])
    lam_d = din("lam", [128, 8])
    wbda_d = din("wbda", [128, 8, 128])
    wbdx_d = din("wbdx", [128, 8, 128])
    gcat_d = din("gcat", [128, 16])
    ln1g_d = din("ln1g", [128, D])
    ln1b_d = din("ln1b", [128, D])
    ln2g_d = din("ln2g", [128, D])
    ln2b_d = din("ln2b", [128, D])
    wr_d = din("wr", [D, 36])
    br_d = din("br", [128, 36])
    ne_in = NE if stop_after is None else 1
    w1_d = din("w1", [ne_in, 128, 16 * DE])
    w3_d = din("w3", [ne_in, 128, 16 * DE])
    w2_d = din("w2", [ne_in, DE, D])
    etab_d = din("etab", [128, 3, NH, 256])
    flag_d = din("flag", [128, 1])
    ident_d = din("ident", [128, 128])
    tri_d = din("tri", [128, 128])
    ebase_d = din("ebase", [128, NE])
    out_d = nc.dram_tensor("out", [HALF, D], F32, kind="ExternalOutput").ap()
    dbg = {}
    if debug:
        dbg["yT"] = nc.dram_tensor("dbg_yT", [128, 16, HALF], F32, kind="ExternalOutput").ap()
        dbg["ssq"] = nc.dram_tensor("dbg_ssq", [128, 32], F32, kind="ExternalOutput").ap()
        dbg["x1"] = nc.dram_tensor("dbg_x1", [HALF, D], F32, kind="ExternalOutput").ap()
        dbg["rt"] = nc.dram_tensor("dbg_rt", [128, 16, 4], F32, kind="ExternalOutput").ap()
    x1s = nc.dram_tensor("x1s", [HALF, D], F32).ap()
    xs_d = nc.dram_tensor("xs", [NE * CAP, D], BF16).ap()
    ys_d = nc.dram_tensor("ys", [NE * CAP, D], F32).ap()

    S = Sched(nc)
    dmaq = ["sync"]
    dctr = [0]

    def hwq():
        dctr[0] += 1
        return dmaq[dctr[0] % len(dmaq)]

    def MM(out, lhsT, rhs, start, stop, r, w):
        S.op("tensor", lambda e: e.matmul(out, lhsT, rhs, start=start, stop=stop), r, w)

    def MMG(out, pairs, r, w):
        n = len(pairs)
        for i, (lhsT, rhs) in enumerate(pairs):
            edge = i == 0 or i == n - 1
            MM(out, lhsT, rhs, i == 0, i == n - 1, r if edge else [], w if edge else [])

    def TR(out, in_, ident, r, w):
        S.op("tensor", lambda e: e.transpose(out, in_, ident), r, w)

    def ACT(out, in_, func, r, w, bias=None, scale=None, accum=None):
        kw = {}
        if bias is not None:
            kw["bias"] = bias
        if scale is not None:
            kw["scale"] = scale
        if accum is not None:
            kw["accum_out"] = accum
        S.op("scalar", lambda e: e.activation(out=out, in_=in_, func=func, **kw), r, w)

    def TS(eng, out, in0, s1, s2, op0, op1, r, w):
        if op1 is None:
            S.op(eng, lambda e: e.tensor_scalar(out, in0, s1, None, op0), r, w)
        else:
            S.op(eng, lambda e: e.tensor_scalar(out, in0, s1, s2, op0, op1), r, w)

    def STT(out, in0, scalar, in1, op0, op1, r, w):
        S.op("vector", lambda e: e.scalar_tensor_tensor(out, in0, scalar, in1, op0, op1), r, w)

    def TT(eng, out, in0, in1, op, r, w):
        S.op(eng, lambda e: e.tensor_tensor(out, in0, in1, op), r, w)

    def CP(eng, out, in_, r, w):
        S.op(eng, lambda e: e.tensor_copy(out=out, in_=in_), r, w)

    def MS(eng, ap, val, w):
        S.op(eng, lambda e: e.memset(ap, val), (), w)

    def REC(out, in_, r, w):
        S.op("vector", lambda e: e.reciprocal(out=out, in_=in_), r, w)

    def RED(out, in_, op, r, w):
        S.op("vector", lambda e: e.tensor_reduce(out=out, in_=in_, axis=AX.X, op=op), r, w)

    def BNS(out, in_, r, w):
        S.op("vector", lambda e: e.bn_stats(out=out, in_=in_), r, w)

    def BNA(out, in_, r, w):
        S.op("vector", lambda e: e.bn_aggr(out=out, in_=in_), r, w)

    def SCAN(out, d0, d1, init, r, w):
        S.op("vector", lambda e: e.tensor_tensor_scan(out=out, data0=d0, data1=d1, initial=init, op0=ALU.mult, op1=ALU.add), r, w)

    bcreg = {}

    def bc(e):
        if "r" not in bcreg:
            bcreg["r"] = e.to_reg(NE * CAP - 1)
        return bcreg["r"]

    def SCAT(dram, idx, src, sem, r, w):
        S.dma("gpsimd", lambda e: e.indirect_dma_start(out=dram, out_offset=bass.IndirectOffsetOnAxis(ap=idx, axis=0), in_=src, in_offset=None,
                                                       bounds_check=bc(e), oob_is_err=False), sem, r, w)

    def GATH(dst, dram, idx, sem, r, w):
        S.dma("gpsimd", lambda e: e.indirect_dma_start(out=dst, out_offset=None, in_=dram, in_offset=bass.IndirectOffsetOnAxis(ap=idx, axis=0),
                                                       bounds_check=bc(e), oob_is_err=False), sem, r, w)

    def DMA(eng, out, in_, sem, r, w, final=False):
        S.dma(eng, lambda e: e.dma_start(out=out, in_=in_), sem, r, w, final=final)

    with contextlib.ExitStack() as top:
        uniq = [0]

        def sb(st, name, shape, dt=F32):
            uniq[0] += 1
            return st.enter_context(nc.sbuf_tensor(f"s{uniq[0]}_{name}", list(shape), dt))

        def ps(st, name, shape, dt=F32):
            uniq[0] += 1
            return st.enter_context(nc.psum_tensor(f"p{uniq[0]}_{name}", list(shape), dt))

        ident_f = sb(top, "ident_f", [128, 128])
        ident_b = sb(top, "ident_b", [128, 128], BF16)
        flag = sb(top, "flag", [128, 1])
        ones_b = sb(top, "ones_b", [128, 128], BF16)
        pones_b = sb(top, "pones_b", [128, 128], BF16)
        ones_f = sb(top, "ones_f", [128, 128])
        onescol_b = sb(top, "onescol_b", [128, 1], BF16)
        ssq = sb(top, "ssq", [128, 32])
        slots_i = sb(top, "slots_i", [128, 16, 2], I32)
        wts = sb(top, "wts", [128, 16, 2])
        DMA("sync", ident_f[:], ident_d, "k1", (), ["ident_f"])
        DMA("sync", flag[:], flag_d, "k2", (), ["flag"])
        CP("vector", ident_b[:], ident_f[:], ["ident_f"], ["ident_b"])
        MS("vector", ones_b[:], 1.0, ["ones_b"])
        MS("vector", ones_f[:], 1.0, ["ones_f"])
        MS("vector", onescol_b[:], 1.0, ["onescol_b"])
        TS("vector", pones_b[:], ones_f[:], flag[:, 0:1], None, ALU.mult, None, ["ones_f", "flag"], ["pones_b"])
        MS("vector", ssq[:], 0.0, ["ssq"])

        with contextlib.ExitStack() as mix:
            mix.callback(S.barrier)
            yT = sb(mix, "yT", [128, 16, HALF], BF16)

            with contextlib.ExitStack() as ph:
                ph.callback(S.barrier)
                wl = sb(ph, "wl", [128, 16, 2048], BF16)
                cw = sb(ph, "cw", [128, 8, 4])
                cb = sb(ph, "cb", [128, 8])
                lba = sb(ph, "lba", [128, 8])
                lbx = sb(ph, "lbx", [128, 8])
                cl = sb(ph, "cl", [128, 8])
                cl2 = sb(ph, "cl2", [128, 8])
                wbda = sb(ph, "wbda", [128, 8, 128], BF16)
                wbdx = sb(ph, "wbdx", [128, 8, 128], BF16)
                stg = [sb(ph, "lstg0", [128, 2048])] * 2
                xw = [sb(ph, f"lxw{i}", [128, 16, LW], BF16) for i in range(2)]
                xr = sb(ph, "xr", [128, 8, LW + 3])
                xc2 = [sb(ph, f"xc{i}", [128, 8, LW]) for i in range(2)]
                xcb2 = [sb(ph, f"xcb{i}", [128, 8, LW], BF16) for i in range(2)]
                gr2 = [sb(ph, "gr0", [128, 8, LW])] * 2
                gi2 = [sb(ph, f"gi{i}", [128, 8, LW]) for i in range(2)]
                ga2 = [sb(ph, f"ga{i}", [128, 8, LW]) for i in range(2)]
                gs2 = [sb(ph, "gs0", [128, 8, LW])] * 2
                hh = [sb(ph, f"hh{i}", [128, 8, LW]) for i in range(2)]
                gg2 = [sb(ph, f"gg{i}", [128, 8, LW]) for i in range(2)]
                pxr = [ps(ph, f"pxr{i}", [128, 2, LW]) for i in range(4)]
                pg = [ps(ph, f"pg{i}", [128, 2, LW]) for i in range(2)]
                pss = ps(ph, "pss", [128, 16])

                for t, d_, kn in ((cw, convw, "cw"), (cb, convb, "cb"), (lba, ba_d, "lba"), (lbx, bx_d, "lbx"), (cl, lam_d, "cl")):
                    DMA("sync", t[:], d_, "c2" + kn, (), [kn])
                ACT(cl[:], cl[:], AF.Exp, ["cl"], ["cl"], scale=-1.0)
                TS("vector", cl2[:], cl[:], -0.25, 1.0 / 3.0, ALU.mult, ALU.add, ["cl"], ["cl2"])
                TT("vector", cl2[:], cl2[:], cl[:], ALU.mult, ["cl", "cl2"], ["cl2"])
                TS("vector", cl2[:], cl2[:], -0.5, None, ALU.add, None, ["cl2"], ["cl2"])
                TT("vector", cl2[:], cl2[:], cl[:], ALU.mult, ["cl", "cl2"], ["cl2"])
                TS("vector", cl2[:], cl2[:], 1.0, None, ALU.add, None, ["cl2"], ["cl2"])
                TT("vector", cl[:], cl2[:], cl[:], ALU.mult, ["cl", "cl2"], ["cl"])
                TS("vector", cl2[:], cl[:], -16.0, None, ALU.mult, None, ["cl"], ["cl2"])
                TS("vector", cl[:], cl[:], -8.0, None, ALU.mult, None, ["cl", "cl2"], ["cl"])
                for t, d_, kn in ((wbda, wbda_d, "wbda"), (wbdx, wbdx_d, "wbdx")):
                    DMA("sync", stg[0][:, 0:1024], d_.rearrange("p c m -> p (c m)"), "c3", [], ["lstg0"])
                    CP("vector", t[:].rearrange("p c m -> p (c m)"), stg[0][:, 0:1024], ["lstg0"], [kn])
                w_in_v = w_in.rearrange("(k p) n -> p k n", p=128)
                pstg = [t_[:].rearrange("p c t -> p (c t)") for t_ in (xc2[0], xc2[1], gi2[0], gi2[1], ga2[0], ga2[1], gg2[0], gg2[1])]
                for k in range(16):
                    for h_ in range(2):
                        i8 = (2 * k + h_) % 8
                        DMA(hwq(), pstg[i8], w_in_v[:, k, 3072 + h_ * 1024:3072 + (h_ + 1) * 1024], f"lp{i8}", [], [("lpst", i8)])
                        if h_:
                            ACT(wl[:, k, h_ * 1024:(h_ + 1) * 1024], pstg[i8], AF.Copy, [("lpst", i8)], [("wl", k, h_)])
                        else:
                            CP("vector", wl[:, k, h_ * 1024:(h_ + 1) * 1024], pstg[i8], [("lpst", i8)], [("wl", k, h_)])
                S.barrier()
                MS("vector", xr[:], 0.0, [("xr", p_) for p_ in range(4)])
                MS("vector", hh[1][:], 0.0, [("hh1", p_) for p_ in range(4)])
                wl_keys = [("wl", k, h_) for k in range(16) for h_ in range(2)]
                def load_xwin(wi_):
                    DMA(hwq(), stg[wi_ % 2][:], xT[wi_], "lw0", [], ["lstg0"])
                    ACT(xw[wi_ % 2][:].rearrange("p k t -> p (k t)"), stg[wi_ % 2][:], AF.Copy, ["lstg0"], [f"lxw{wi_ % 2}"])

                NWL = 0 if skip_lru else 2 * HALF // LW
                if NWL:
                    load_xwin(0)
                def lru_window(wi):
                    own = wi >= HALF // LW
                    s_ = wi % 2
                    t0 = wi * LW
                    o0 = t0 - HALF
                    wp = wi % 2
                    xc_w, xcb_w, gr_w, gi_w, ga_w, gs_w, gg_w = xc2[wp], xcb2[wp], gr2[wp], gi2[wp], ga2[wp], gs2[wp], gg2[wp]
                    sq_w = xcb_w
                    hcur, hprev = hh[wi % 2], hh[(wi + 1) % 2]
                    hk, hpk = f"hh{wi % 2}", f"hh{(wi + 1) % 2}"
                    for p in range(4):
                        CP("vector", xr[:, 2 * p:2 * p + 2, 0:3], xr[:, 2 * p:2 * p + 2, LW:LW + 3], [("xr", p)], [("xr", p)])
                        for c in (2 * p, 2 * p + 1):
                            MMG(pxr[p][:, c % 2, :], [(wl[:, k, c * 128:(c + 1) * 128], xw[s_][:, k, :]) for k in range(16)],
                                wl_keys + [f"lxw{s_}"], [("pxr", p)])
                        CP("vector", xr[:, 2 * p:2 * p + 2, 3:3 + LW], pxr[p][:, :, :], [("pxr", p)], [("xr", p)])
                    yield
                    if own:
                        for p in range(4):
                            for c in (2 * p, 2 * p + 1):
                                MMG(pxr[p][:, c % 2, :], [(wl[:, k, 1024 + c * 128:1024 + (c + 1) * 128], xw[s_][:, k, :]) for k in range(16)],
                                    wl_keys + [f"lxw{s_}"], [("pxr", p)])
                            ACT(gg_w[:, 2 * p:2 * p + 2, :], pxr[p][:, :, :], AF.Gelu_apprx_tanh, [("pxr", p)], [("gg", wp, p)])
                    yield
                    if wi + 1 < NWL:
                        load_xwin(wi + 1)
                    for p in range(4):
                        for c in (2 * p, 2 * p + 1):
                            TS("vector", xc_w[:, c, :], xr[:, c, 3:3 + LW], cw[:, c, 3:4], cb[:, c:c + 1], ALU.mult, ALU.add,
                               [("xr", p), "cw", "cb"], [("xcc", wp, c)])
                        for j in range(3):
                            for c in (2 * p, 2 * p + 1):
                                STT(xc_w[:, c, :], xr[:, c, j:j + LW], cw[:, c, j:j + 1], xc_w[:, c, :], ALU.mult, ALU.add, [("xr", p), ("xcc", wp, c), "cw"], [("xcc", wp, c)])
                        ACT(xcb_w[:, 2 * p:2 * p + 2, :], xc_w[:, 2 * p:2 * p + 2, :], AF.Copy, [("xcc", wp, 2 * p), ("xcc", wp, 2 * p + 1)], [("xcb", wp, p)])
                    yield
                    for p in range(4):
                        for c in (2 * p, 2 * p + 1):
                            pa = pg[c % 2]
                            MM(pa[:, 0, :], wbda[:, c, :], xcb_w[:, c, :], True, True, ["wbda", ("xcb", wp, p)], [("pg", c % 2)])
                            MM(pa[:, 1, :], wbdx[:, c, :], xcb_w[:, c, :], True, True, ["wbdx", ("xcb", wp, p)], [("pg", c % 2)])
                            ACT(gr_w[:, c, :], pa[:, 0, :], AF.Sigmoid, [("pg", c % 2), "lba"], [("gr", 0, p)], bias=lba[:, c:c + 1])
                            ACT(gi_w[:, c, :], pa[:, 1, :], AF.Sigmoid, [("pg", c % 2), "lbx"], [("gi", wp, p)], bias=lbx[:, c:c + 1])
                    yield
                    for p in range(4):
                        for c in (2 * p, 2 * p + 1):
                            ACT(ga_w[:, c, :], gr_w[:, c, :], AF.Exp, [("gr", 0, p), "cl"], [("ga", wp, p)], scale=cl[:, c:c + 1])
                            ACT(gs_w[:, c, :], gr_w[:, c, :], AF.Exp, [("gr", 0, p), "cl2"], [("gs", 0, p)], scale=cl2[:, c:c + 1])
                    yield
                    for p in range(4):
                        sl2 = slice(2 * p, 2 * p + 2)
                        ACT(gs_w[:, sl2, :], gs_w[:, sl2, :], AF.Sqrt, [("gs", 0, p)], [("gs", 0, p)], bias=1.0, scale=-1.0)
                    yield
                    for p in range(4):
                        sl2 = slice(2 * p, 2 * p + 2)
                        TT("vector", gi_w[:, sl2, :], gi_w[:, sl2, :], xc_w[:, sl2, :], ALU.mult, [("gi", wp, p), ("xcc", wp, 2 * p), ("xcc", wp, 2 * p + 1)], [("gi", wp, p)])
                        if own:
                            TT("vector", gs_w[:, sl2, :], gs_w[:, sl2, :], gi_w[:, sl2, :], ALU.mult, [("gs", 0, p), ("gi", wp, p)], [("gs", 0, p)])
                        else:
                            STT(gs_w[:, sl2, :], gi_w[:, sl2, :], flag[:, 0:1], gs_w[:, sl2, :], ALU.mult, ALU.mult, [("gs", 0, p), ("gi", wp, p), "flag"], [("gs", 0, p)])
                        for c in (2 * p, 2 * p + 1):
                            SCAN(hcur[:, c, :], ga_w[:, c, :], gs_w[:, c, :], hprev[:, c, LW - 1:LW], [("ga", wp, p), ("gs", 0, p), (hpk, p)], [(hk, p)])
                        if own:
                            TT("vector", gg_w[:, sl2, :], gg_w[:, sl2, :], hcur[:, sl2, :], ALU.mult, [("gg", wp, p), (hk, p)], [("gg", wp, p)])
                            ACT(yT[:, 8 + 2 * p:10 + 2 * p, o0:o0 + LW], gg_w[:, sl2, :], AF.Copy, [("gg", wp, p)], [("yT", "l", wi)])
                            TT("vector", sq_w[:, sl2, :], gg_w[:, sl2, :], gg_w[:, sl2, :], ALU.mult, [("gg", wp, p)], [("xcb", wp, p)])
                    if own:
                        for tb in range(LW // 128):
                            tile_i = (o0 + tb * 128) // 128
                            MMG(pss[:, tile_i:tile_i + 1], [(sq_w[:, c, tb * 128:(tb + 1) * 128], onescol_b[:, 0:1]) for c in range(8)],
                                [("xcb", wp, p_) for p_ in range(4)] + ["onescol_b"], ["pss"])

                gens = [lru_window(w_) for w_ in range(NWL)]
                if NWL:
                    for _ in range(3):
                        next(gens[0], None)
                for w_ in range(NWL):
                    nxt = gens[w_ + 1] if w_ + 1 < NWL else None
                    for step in range(4):
                        next(gens[w_], None)
                        if nxt is not None and step < 3:
                            next(nxt, None)
                    for _ in gens[w_]:
                        pass
                if not skip_lru:
                    CP("vector", ssq[:, 16:32], pss[:, 0:16], ["pss"], ["ssq"])
                else:
                    for w_ in range(HALF // LW, 2 * HALF // LW):
                        MS("gpsimd", yT[:, 8:16, (w_ - HALF // LW) * LW:(w_ - HALF // LW + 1) * LW], 0.0, [("yT", "l", w_)])

            if stop_after == "lru":
                pass
            else:
                with contextlib.ExitStack() as ph:
                    ph.callback(S.barrier)
                    accO = sb(ph, "accO", [128, 2, HALF])
                    accD = sb(ph, "accD", [128, 2, HALF])
                    etb = sb(ph, "etb", [128, 3, 4, 256], BF16)
                    qTm = sb(ph, "qTm", [128, 4, HALF], BF16)
                    kT = sb(ph, "kT", [128, 2, 2 * HALF], BF16)
                    vT = sb(ph, "vT", [128, 2, 2 * HALF], BF16)
                    ALV = {"a1": 1, "a2": 2, "a2p0": 2, "a2p1": 2, "a2p2": 2, "a3": 3, "a3p0": 3}.get(stop_after, 9)
                    VPAT = {"a2p0": (0,), "a2p1": (1,), "a2p2": (2,)}.get(stop_after, (0, 1, 2))
                    APAT = PATTERNS[:1] if stop_after == "a3p0" else PATTERNS
                    for pas in range(4 if ALV == 9 else 1):
                        MS("vector", qTm[:], 0.0, ["qTm"])
                        with contextlib.ExitStack() as ip:
                            ip.callback(S.barrier)
                            wq = sb(ip, "wq", [128, 16, 768], BF16)
                            stg = [sb(ip, f"astg{i}", [128, 16 * AW]) for i in range(2)]
                            xw = [sb(ip, f"axw{i}", [128, 16, AW], BF16) for i in range(2)]
                            pq = [ps(ip, f"pq{i}", [128, 2, AW]) for i in range(3)]
                            for pi_ in range(3):
                                s_ = pi_ % 2
                                DMA(hwq(), stg[s_][:, 0:1024].rearrange("p (h q) -> p h q", h=4), etab_d[:, pi_, pas * 4:(pas + 1) * 4, :],
                                    f"aw{s_}", [], [f"astg{s_}"])
                                ACT(etb[:, pi_, :, :], stg[s_][:, 0:1024].rearrange("p (h q) -> p h q", h=4), AF.Copy, [f"astg{s_}"], ["etb"])
                            wq_flat = wq[:].rearrange("p k n -> p (k n)")
                            for i6 in range(6):
                                s_ = i6 % 2
                                DMA(hwq(), stg[s_][:], wqkv_d[pas][:, i6 * 2048:(i6 + 1) * 2048], f"aw{s_}", [], [f"astg{s_}"])
                                if i6 % 2:
                                    ACT(wq_flat[:, i6 * 2048:(i6 + 1) * 2048], stg[s_][:], AF.Copy, [f"astg{s_}"], [("wq", i6)])
                                else:
                                    CP("vector", wq_flat[:, i6 * 2048:(i6 + 1) * 2048], stg[s_][:], [f"astg{s_}"], [("wq", i6)])
                            def load_xwin_a(wi_):
                                q_ = wi_ % 2
                                hf = 8 * AW
                                DMA(hwq(), stg[q_][:], xT[wi_], f"aw{q_}", [], [f"astg{q_}"])
                                ACT(xw[q_][:].rearrange("p k t -> p (k t)")[:, 0:hf], stg[q_][:, 0:hf], AF.Copy, [f"astg{q_}"], [(f"axw{q_}", 0)])
                                ACT(xw[q_][:].rearrange("p k t -> p (k t)")[:, hf:2 * hf], stg[q_][:, hf:2 * hf], AF.Copy, [f"astg{q_}"], [(f"axw{q_}", 1)])

                            load_xwin_a(0)
                            for wi in range(2 * HALF // AW):
                                own = wi >= HALF // AW
                                s_ = wi % 2
                                t0 = wi * AW
                                if wi + 1 < 2 * HALF // AW:
                                    load_xwin_a(wi + 1)
                                for j in ((1, 2, 0) if own else (1, 2)):
                                    pt = pq[j]
                                    for cc in range(2):
                                        col = j * 256 + cc * 128
                                        MMG(pt[:, cc, :], [(wq[:, k, col:col + 128], xw[s_][:, k, :]) for k in range(16)],
                                            [("wq", k) for k in range(6)] + [(f"axw{s_}", 0), (f"axw{s_}", 1)], [("pq", j)])
                                    if j == 1:
                                        CP("vector", kT[:, :, t0:t0 + AW], pt[:, :, :], [("pq", j)], ["kT"])
                                    elif j == 2:
                                        ACT(vT[:, :, t0:t0 + AW], pt[:, :, :], AF.Copy, [("pq", j)], ["vT"])
                                    else:
                                        o0 = t0 - HALF
                                        for hl in range(4):
                                            po = (hl % 2) * 64
                                            ACT(qTm[po:po + 64, hl, o0:o0 + AW], pt[po:po + 64, hl // 2, :], AF.Copy,
                                                [("pq", j)], ["qTm"], scale=0.125)
                        with contextlib.ExitStack() as at:
                            at.callback(S.barrier)
                            NVB = 69
                            vtok = sb(at, "vtok", [128, NVB, 256], BF16)
                            PT = [sb(at, f"PT{i}", [128, 256], BF16) for i in range(6)]
                            EX = [sb(at, f"EX{i}", [128, 256], BF16) for i in range(6)]
                            sqa = sb(at, "sqa", [128, 2, HALF], BF16)
                            vt = contextlib.ExitStack()
                            ptr = [ps(vt, f"ptr{i}", [128, 256], BF16) for i in range(6)]
                            vmap = {}
                            nv = 0
                            for pi, d in enumerate(PATTERNS if ALV >= 2 else ()):
                                if pi not in VPAT:
                                    continue
                                nblk = 32 // d
                                for r_ in range(d):
                                    for b in range(nblk // 2 - 1, nblk):
                                        vmap[(pi, r_, b)] = nv
                                        st_ = d * 128 * b + r_
                                        pt = ptr[nv % 6]
                                        for cc in range(2):
                                            TR(pt[:, cc * 128:(cc + 1) * 128], vT[:, cc, st_:st_ + d * 127 + 1:d], ident_b[:],
                                               ["vT", "ident_b"], [("ptr", nv % 6)])
                                        if nv % 2:
                                            ACT(vtok[:, nv, :], pt[:], AF.Copy, [("ptr", nv % 6)], [("vtok", nv)])
                                        else:
                                            CP("vector", vtok[:, nv, :], pt[:], [("ptr", nv % 6)], [("vtok", nv)])
                                        nv += 1
                            assert nv == NVB or ALV < 9
                            S.barrier()
                            vt.close()
                            pST = [ps(at, f"pST{i}", [128, 256]) for i in range(4)]
                            pO = [ps(at, f"pO{i}", [128, 512]) for i in range(2)]
                            pD = [ps(at, f"pD{i}", [128, 512]) for i in range(2)]
                            items = []
                            gh_ = 0
                            for pi, d in enumerate(APAT if ALV >= 3 else ()):
                                nblk = 32 // d
                                groups = []
                                if d == 1:
                                    for g in range(4):
                                        groups.append(([(0, 16 + 4 * g + u) for u in range(4)],
                                                       lambda a, g=g: a[:, 512 * g:512 * (g + 1)]))
                                elif d == 4:
                                    for c_ in range(4):
                                        groups.append(([(c_, 4 + u) for u in range(4)],
                                                       lambda a, c_=c_: a[:, c_:HALF:4]))
                                else:
                                    for g in range(4):
                                        groups.append(([(4 * g + u, 1) for u in range(4)],
                                                       lambda a, g=g: a.rearrange("p (a r) -> p r a", r=16)[:, 4 * g:4 * g + 4, :]))
                                for units, accview in groups:
                                    for hl in range(4):
                                        for u, (r_, n) in enumerate(units):
                                            items.append((pi, d, nblk, hl, gh_, u, r_, n, accview, u == 3))
                                        gh_ += 1
                            NST, NPT, SKEW = 4, 6, 3

                            def unit_front(i, it):
                                pi, d, nblk, hl, gh, u, r_, n, accview, last = it
                                cc = hl // 2
                                qs = d * 128 * n + r_ - HALF
                                qap = qTm[:, hl, qs:qs + d * 127 + 1:d]
                                sti, pti = i % NST, i % NPT
                                for j, b in enumerate((n - 1, n)):
                                    ks = d * 128 * b + r_
                                    MM(pST[sti][:, j * 128:(j + 1) * 128], kT[:, cc, ks:ks + d * 127 + 1:d], qap, True, True,
                                       ["kT", "qTm"], [("pST", sti)])
                                ACT(EX[pti][:], pST[sti][:], AF.Exp, [("pST", sti)], [("EX", pti)])
                                TT("vector", PT[pti][:], EX[pti][:], etb[:, pi, hl, :], ALU.mult, [("EX", pti), "etb"], [("PT", pti)])

                            pend = []

                            def unit_back(i, it):
                                while pend:
                                    pend.pop(0)()
                                pi, d, nblk, hl, gh, u, r_, n, accview, last = it
                                cc, po = hl // 2, (hl % 2) * 64
                                go, pti = gh % 2, i % NPT
                                for j, b in enumerate((n - 1, n)):
                                    vi = vmap[(pi, r_, b)]
                                    prev_half = b < nblk // 2
                                    MM(pO[go][:, u * 128:(u + 1) * 128], vtok[:, vi, cc * 128:(cc + 1) * 128], PT[pti][:, j * 128:(j + 1) * 128],
                                       j == 0, j == 1, [("vtok", vi), ("PT", pti)], [("pO", go)])
                                    MM(pD[go][:, u * 128:(u + 1) * 128], (pones_b if prev_half else ones_b)[:], PT[pti][:, j * 128:(j + 1) * 128],
                                       j == 0, j == 1, ["pones_b", "ones_b", ("PT", pti)], [("pD", go)])
                                if not last:
                                    return
                                ao = accview(accO[po:po + 64, cc, :])
                                ad = accview(accD[po:po + 64, cc, :])
                                so = pO[go][po:po + 64, :]
                                sd = pD[go][po:po + 64, :]
                                if d == 16:
                                    so = so.rearrange("p (u q) -> p u q", u=4)
                                    sd = sd.rearrange("p (u q) -> p u q", u=4)
                                if pi == 0:
                                    ACT(ao, so, AF.Copy, [("pO", go)], [("accO", hl)])
                                    pend.append(lambda: ACT(ad, sd, AF.Copy, [("pD", go)], [("accD", hl)]))
                                else:
                                    TT("vector", ao, so, ao, ALU.add, [("pO", go), ("accO", hl)], [("accO", hl)])
                                    pend.append(lambda: TT("vector", ad, sd, ad, ALU.add, [("pD", go), ("accD", hl)], [("accD", hl)]))

                            for i in range(len(items) + SKEW if items else 0):
                                if i < len(items):
                                    unit_front(i, items[i])
                                if i >= SKEW:
                                    unit_back(i - SKEW, items[i - SKEW])
                            while pend:
                                pend.pop(0)()
                            acck = [("accO", h_) for h_ in range(4)] + [("accD", h_) for h_ in range(4)]
                            for cc in range(2 if ALV >= 9 else 0):
                                ACT(accD[:, cc, :], accD[:, cc, :], AF.Ln, acck, acck)
                                ACT(accD[:, cc, :], accD[:, cc, :], AF.Exp, acck, acck, scale=-1.0)
                                TT("vector", accO[:, cc, :], accO[:, cc, :], accD[:, cc, :], ALU.mult, acck, acck)
                                ACT(yT[:, pas * 2 + cc, :], accO[:, cc, :], AF.Copy, acck, [("yT", "a", pas)])
                                TT("vector", sqa[:, cc, :], accO[:, cc, :], accO[:, cc, :], ALU.mult, acck, ["sqa"])
                            if ALV >= 9:
                                S.barrier()
                                psa = pST[0]
                            for ti in range(16 if ALV >= 9 else 0):
                                MMG(psa[:, ti:ti + 1], [(sqa[:, cc, ti * 128:(ti + 1) * 128], onescol_b[:, 0:1]) for cc in range(2)],
                                    ["sqa", "onescol_b"], ["psa"])
                            if ALV >= 9:
                                TT("vector", ssq[:, 0:16], psa[:, 0:16], ssq[:, 0:16], ALU.add, ["psa", "ssq"], ["ssq"])

            if debug:
                with contextlib.ExitStack() as dd:
                    dd.callback(S.barrier)
                    dtmp = sb(dd, "dtmp", [128, 16, HALF])
                    CP("vector", dtmp[:], yT[:], [("yT", "l", w_) for w_ in range(HALF // LW, 2 * HALF // LW)] + [("yT", "a", p_) for p_ in range(4)], ["dtmp"])
                    DMA("sync", dbg["yT"], dtmp[:], "dbg", ["dtmp"], [], final=True)
                    DMA("sync", dbg["ssq"], ssq[:], "dbg", ["ssq"], [], final=True)

            yT_keys = [("yT", "l", w_) for w_ in range(HALF // LW, 2 * HALF // LW)] + [("yT", "a", p_) for p_ in range(4)]

            if stop_after in ("lru", "mixer", "a1", "a2", "a2p0", "a2p1", "a2p2", "a3", "a3p0"):
                pass
            else:
                with contextlib.ExitStack() as ph:
                    ph.callback(S.barrier)
                    wo = sb(ph, "wo", [128, 16, D], BF16)
                    gcat = sb(ph, "gcat", [128, 16])
                    g1 = sb(ph, "g1", [128, D])
                    b1 = sb(ph, "b1", [128, D])
                    wr = sb(ph, "wr", [128, 16, 36])
                    brt = sb(ph, "brt", [128, 36])
                    tri = sb(ph, "tri", [128, 128])
                    ebase = sb(ph, "ebase", [128, NE])
                    cm = sb(ph, "cm", [128, NE])
                    rstd = sb(ph, "rstd", [128, 32])
                    DMA("sync", gcat[:], gcat_d, "k3", [], ["gcat"])
                    DMA("sync", g1[:], ln1g_d, "k4", [], ["g1"])
                    DMA("sync", b1[:], ln1b_d, "k5", [], ["b1"])
                    DMA("sync", wr[:], wr_d.rearrange("(k p) n -> p k n", p=128), "k6", [], ["wr"])
                    DMA("sync", brt[:], br_d, "k7", [], ["brt"])
                    DMA("sync", tri[:], tri_d, "k8", [], ["tri"])
                    DMA("sync", ebase[:], ebase_d, "k9", [], ["ebase"])
                    MS("vector", cm[:], 0.0, ["cm"])
                    TS("vector", rstd[:], ssq[:], 1.0 / 1024.0, RMS_EPS, ALU.mult, ALU.add, ["ssq"], ["rstd"])
                    ACT(rstd[:], rstd[:], AF.Sqrt, ["rstd"], ["rstd"])
                    REC(rstd[:], rstd[:], ["rstd"], ["rstd"])
                    with contextlib.ExitStack() as wp:
                        wp.callback(S.barrier)
                        stg = [sb(wp, f"ostg{i}", [128, D]) for i in range(4)]
                        w_out_v = w_out.rearrange("(k p) n -> p k n", p=128)
                        for k in range(16):
                            s_ = k % 4
                            DMA(hwq(), stg[s_][:], w_out_v[:, k, :], f"ow{s_}", [], [f"ostg{s_}"])
                            if k % 2:
                                ACT(wo[:, k, :], stg[s_][:], AF.Copy, [f"ostg{s_}", "gcat"], [("wo", k)], scale=gcat[:, k:k + 1])
                            else:
                                TS("vector", wo[:, k, :], stg[s_][:], gcat[:, k:k + 1], None, ALU.mult, None,
                                   [f"ostg{s_}", "gcat"], [("wo", k)])
                    with contextlib.ExitStack() as tl:
                        tl.callback(S.barrier)
                        zb = [sb(tl, f"zb{i}", [128, D]) for i in range(2)]
                        x1 = sb(tl, "x1", [128, D])
                        x1b = sb(tl, "x1b", [128, D], BF16)
                        x1T = sb(tl, "x1T", [128, 16, 128])
                        st6s = [sb(tl, f"st6{i}", [128, 4, 6]) for i in range(2)]
                        mv = sb(tl, "mv", [128, 2])
                        sm = sb(tl, "sm", [128, 16])
                        lg = sb(tl, "lg", [128, 36])
                        lm = sb(tl, "lm", [128, NE])
                        lm2 = sb(tl, "lm2", [128, NE])
                        mk1 = sb(tl, "mk1", [128, NE])
                        mk2 = sb(tl, "mk2", [128, NE])
                        mk = sb(tl, "mk", [128, NE])
                        slot = sb(tl, "slot", [128, NE])
                        ovf = sb(tl, "ovf", [128, NE])
                        g4 = sb(tl, "g4", [128, 4])
                        oh4 = sb(tl, "oh4", [128, 4])
                        slf = sb(tl, "slf", [128, 2])
                        pA = [ps(tl, f"pA{i}", [128, 512]) for i in range(2)]
                        pL = [ps(tl, f"pL{i}", [128, 512]) for i in range(2)]
                        pT = [ps(tl, f"pT{i}", [128, 512]) for i in range(2)]
                        pR = ps(tl, "pR", [128, 36])
                        pC = ps(tl, "pC", [128, NE])
                        wo_keys = [("wo", k) for k in range(16)]
                        def part_a(ti):
                            z = zb[ti % 2]
                            zk = f"zb{ti % 2}"
                            tk0 = ti * 128
                            st6 = st6s[ti % 2]
                            s6k = f"st6{ti % 2}"
                            DMA(hwq(), z[:], xtok[tk0:tk0 + 128, :], f"xt{ti % 2}", [], [zk])
                            ACT(z[:], z[:], AF.Copy, [zk], [zk], scale=ALPHA)
                            for nb_ in range(4):
                                b_ = (ti * 4 + nb_) % 2
                                MMG(pA[b_][:], [(yT[:, k, tk0:tk0 + 128], wo[:, k, nb_ * 512:(nb_ + 1) * 512]) for k in range(8)],
                                    yT_keys + wo_keys, [("pA", b_)])
                                MMG(pL[b_][:], [(yT[:, k, tk0:tk0 + 128], wo[:, k, nb_ * 512:(nb_ + 1) * 512]) for k in range(8, 16)],
                                    yT_keys + wo_keys, [("pL", b_)])
                                zs = z[:, nb_ * 512:(nb_ + 1) * 512]
                                STT(zs, pA[b_][:], rstd[:, ti:ti + 1], zs, ALU.mult, ALU.add, [("pA", b_), "rstd", zk], [zk])
                                STT(zs, pL[b_][:], rstd[:, 16 + ti:17 + ti], zs, ALU.mult, ALU.add, [("pL", b_), "rstd", zk], [zk])
                                BNS(st6[:, nb_, :], zs, [zk], [s6k])
                                yield
                        def part_b(ti):
                            z = zb[ti % 2]
                            zk = f"zb{ti % 2}"
                            tk0 = ti * 128
                            st6 = st6s[ti % 2]
                            s6k = f"st6{ti % 2}"
                            BNA(mv[:], st6[:].rearrange("p a b -> p (a b)"), [s6k], ["mv"])
                            TS("vector", sm[:, 0:1], mv[:, 1:2], LN_EPS, None, ALU.add, None, ["mv"], ["sm"])
                            ACT(sm[:, 0:1], sm[:, 0:1], AF.Sqrt, ["sm"], ["sm"])
                            REC(sm[:, 0:1], sm[:, 0:1], ["sm"], ["sm"])
                            STT(sm[:, 1:2], mv[:, 0:1], -1.0, sm[:, 0:1], ALU.mult, ALU.mult, ["mv", "sm"], ["sm"])
                            ACT(x1[:], z[:], AF.Identity, [zk, "sm"], ["x1"], bias=sm[:, 1:2], scale=sm[:, 0:1])
                            TT("vector", x1[:], x1[:], g1[:], ALU.mult, ["x1", "g1"], ["x1"])
                            TT("vector", x1[:], x1[:], b1[:], ALU.add, ["x1", "b1"], ["x1"])
                            DMA("sync", x1s[tk0:tk0 + 128, :], x1[:], "x1st", ["x1"], [("x1s", ti)])
                            if debug:
                                DMA("sync", dbg["x1"][tk0:tk0 + 128, :], x1[:], "dbg", ["x1"], [], final=True)
                            ACT(x1b[:], x1[:], AF.Copy, ["x1"], ["x1b"])
                            yield
                            for k4 in range(4):
                                pt = pT[k4 % 2]
                                for kk in range(4):
                                    k = k4 * 4 + kk
                                    TR(pt[:, kk * 128:(kk + 1) * 128], x1[:, k * 128:(k + 1) * 128], ident_f[:], ["x1", "ident_f"], [("pT", k4 % 2)])
                                if k4 % 2:
                                    ACT(x1T[:, k4 * 4:(k4 + 1) * 4, :], pt[:].rearrange("p (a b) -> p a b", a=4), AF.Copy, [("pT", k4 % 2)], ["x1T"])
                                else:
                                    CP("vector", x1T[:, k4 * 4:(k4 + 1) * 4, :], pt[:].rearrange("p (a b) -> p a b", a=4), [("pT", k4 % 2)], ["x1T"])
                            MMG(pR[:], [(x1T[:, k, :], wr[:, k, :]) for k in range(16)], ["x1T", "wr"], ["pR"])
                            TT("vector", lg[:], pR[:], brt[:], ALU.add, ["pR", "brt"], ["lg"])
                            yield
                            RED(sm[:, 2:3], lg[:, 0:4], ALU.max, ["lg"], ["sm"])
                            TS("vector", sm[:, 3:4], sm[:, 2:3], -1.0, None, ALU.mult, None, ["sm"], ["sm"])
                            ACT(g4[:], lg[:, 0:4], AF.Exp, ["lg", "sm"], ["g4", "sm"], bias=sm[:, 3:4], accum=sm[:, 4:5])
                            REC(sm[:, 5:6], sm[:, 4:5], ["sm"], ["sm"])
                            TS("vector", oh4[:], lg[:, 0:4], sm[:, 2:3], None, ALU.is_equal, None, ["lg", "sm"], ["oh4"])
                            TS("vector", oh4[:], oh4[:], -1.0, 1.0e30, ALU.add, ALU.mult, ["oh4"], ["oh4"])
                            for g in range(4):
                                TS("vector", lm[:, 8 * g:8 * g + 8], lg[:, 4 + 8 * g:12 + 8 * g], oh4[:, g:g + 1], None, ALU.add, None,
                                   ["lg", "oh4"], ["lm"])
                            RED(sm[:, 6:7], lm[:], ALU.max, ["lm"], ["sm"])
                            TS("vector", mk1[:], lm[:], sm[:, 6:7], None, ALU.is_equal, None, ["lm", "sm"], ["mk1"])
                            STT(lm2[:], mk1[:], -1.0e30, lm[:], ALU.mult, ALU.add, ["mk1", "lm"], ["lm2"])
                            RED(sm[:, 7:8], lm2[:], ALU.max, ["lm2"], ["sm"])
                            TS("vector", mk2[:], lm2[:], sm[:, 7:8], None, ALU.is_equal, None, ["lm2", "sm"], ["mk2"])
                            TT("vector", sm[:, 8:9], sm[:, 6:7], sm[:, 7:8], ALU.subtract, ["sm"], ["sm"])
                            ACT(sm[:, 9:10], sm[:, 8:9], AF.Sigmoid, ["sm"], ["sm"])
                            ACT(sm[:, 10:11], sm[:, 8:9], AF.Sigmoid, ["sm"], ["sm"], scale=-1.0)
                            TT("vector", wts[:, ti, 0:1], sm[:, 9:10], sm[:, 5:6], ALU.mult, ["sm"], ["wts"])
                            TT("vector", wts[:, ti, 1:2], sm[:, 10:11], sm[:, 5:6], ALU.mult, ["sm"], ["wts"])
                            TT("vector", mk[:], mk1[:], mk2[:], ALU.add, ["mk1", "mk2"], ["mk"])
                            yield
                            MM(pC[:], tri[:], mk[:], True, False, ["tri", "mk"], ["pC"])
                            MM(pC[:], ones_f[:], cm[:], False, True, ["ones_f", "cm"], ["pC"])
                            TS("vector", ovf[:], pC[:], CAP - 0.5, BIG, ALU.is_ge, ALU.mult, ["pC"], ["ovf"])
                            TT("vector", slot[:], pC[:], ebase[:], ALU.add, ["pC", "ebase"], ["slot"])
                            TT("vector", slot[:], slot[:], ovf[:], ALU.add, ["slot", "ovf"], ["slot"])
                            TT("vector", cm[:], cm[:], mk[:], ALU.add, ["cm", "mk"], ["cm"])
                            TT("vector", mk1[:], mk1[:], slot[:], ALU.mult, ["mk1", "slot"], ["mk1"])
                            TT("vector", mk2[:], mk2[:], slot[:], ALU.mult, ["mk2", "slot"], ["mk2"])
                            RED(slf[:, 0:1], mk1[:], ALU.add, ["mk1"], ["slf"])
                            RED(slf[:, 1:2], mk2[:], ALU.add, ["mk2"], ["slf"])
                            CP("vector", slots_i[:, ti, :], slf[:], ["slf"], [("slots", ti)])
                            if debug:
                                CP("vector", sm[:, 12:14], slf[:], ["slf"], ["sm"])
                                CP("vector", sm[:, 14:16], wts[:, ti, :], ["wts", "sm"], ["sm"])
                                DMA("sync", dbg["rt"][:, ti, :], sm[:, 12:16], "dbg", ["sm"], [], final=True)
                            for a_ in range(2):
                                SCAT(xs_d, slots_i[:, ti, a_:a_ + 1], x1b[:], "scat", ["x1b", ("slots", ti)], [("xs", ti, a_)])

                        for _ in part_a(0):
                            pass
                        for ti in range(1, 16):
                            gb = part_b(ti - 1)
                            for _ in part_a(ti):
                                next(gb, None)
                            for _ in gb:
                                pass
                        for _ in part_b(15):
                            pass

        if stop_after in ("lru", "mixer", "ln1", "a1", "a2", "a2p0", "a2p1", "a2p2", "a3", "a3p0"):
            with contextlib.ExitStack() as dd:
                dd.callback(S.barrier)
                zt = sb(dd, "zt", [128, D])
                MS("vector", zt[:], 0.0, ["zt"])
                for ti in range(16):
                    DMA("sync", out_d[ti * 128:(ti + 1) * 128, :], zt[:], "outst", ["zt"], [], final=True)
        else:
            with contextlib.ExitStack() as ph:
                ph.callback(S.barrier)
                w1b = [sb(ph, f"w1b{i}", [128, 16, DE], BF16) for i in range(2)]
                w3b = [sb(ph, f"w3b{i}", [128, 16, DE], BF16) for i in range(2)]
                w2b = [sb(ph, f"w2b{i}", [128, 4, D], BF16) for i in range(2)]
                stg = [sb(ph, f"estg{i}", [128, 2048]) for i in range(NSTG)]
                xe = sb(ph, "xe", [128, 2, D], BF16)
                xeT = sb(ph, "xeT", [128, 16, CAP], BF16)
                h1 = sb(ph, "h1", [128, CAP])
                hT = sb(ph, "hT", [128, 4, CAP], BF16)
                yo = [sb(ph, f"yo{i}", [128, D]) for i in range(2)]
                ptr = [ps(ph, f"etr{i}", [128, 512], BF16) for i in range(2)]
                ph1 = [ps(ph, f"ph1{i}", [128, CAP]) for i in range(2)]
                ph3 = [ps(ph, f"ph3{i}", [128, CAP]) for i in range(2)]
                py = [ps(ph, f"py{i}", [128, 512]) for i in range(2)]
                sctr = [0]
                cast_engs = ["vector", "scalar", "vector", "gpsimd", "scalar", "vector", "scalar", "vector", "gpsimd", "vector", "scalar", "gpsimd"]

                def load_expert(e_):
                    s_ = e_ % 2
                    for wt, src, nk in ((w1b[s_], w1_d[e_], 16), (w3b[s_], w3_d[e_], 16)):
                        for k4 in range(4):
                            si = sctr[0] % NSTG
                            sctr[0] += 1
                            DMA(hwq(), stg[si][:], src[:, k4 * 4 * DE:(k4 + 1) * 4 * DE], f"ew{si}", [], [f"estg{si}"])
                            ce = cast_engs[sctr[0] % 12]
                            dst = wt[:, k4 * 4:(k4 + 1) * 4, :].rearrange("p k n -> p (k n)")
                            if ce == "scalar":
                                ACT(dst, stg[si][:], AF.Copy, [f"estg{si}"], [(wt.name, k4)])
                            else:
                                CP(ce, dst, stg[si][:], [f"estg{si}"], [(wt.name, k4)])
                            yield
                    src = w2_d[e_].rearrange("(k p) n -> p k n", p=128)
                    for k in range(4):
                        si = sctr[0] % NSTG
                        sctr[0] += 1
                        DMA(hwq(), stg[si][:], src[:, k, :], f"ew{si}", [], [f"estg{si}"])
                        ce = cast_engs[sctr[0] % 12]
                        if ce == "scalar":
                            ACT(w2b[s_][:, k, :], stg[si][:], AF.Copy, [f"estg{si}"], [(w2b[s_].name, k)])
                        else:
                            CP(ce, w2b[s_][:, k, :], stg[si][:], [f"estg{si}"], [(w2b[s_].name, k)])
                        yield

                ldr = [iter(())]

                def pump(n):
                    for _ in range(n):
                        next(ldr[0], None)

                for _ in load_expert(0):
                    pass
                for e_ in range(NE):
                    s_ = e_ % 2
                    ldr[0] = load_expert(e_ + 1) if e_ + 1 < NE else iter(())
                    r0 = e_ * CAP
                    DMA("sync", xe[:], xs_d[r0:r0 + CAP, :].rearrange("(b p) d -> p b d", p=128), "xe", [("xs", t_, a2) for t_ in range(16) for a2 in range(2)], ["xe"])
                    pump(2)
                    for b in range(2):
                        for k4 in range(4):
                            pt = ptr[(b * 4 + k4) % 2]
                            pk = ("etr", (b * 4 + k4) % 2)
                            for kk in range(4):
                                k = k4 * 4 + kk
                                TR(pt[:, kk * 128:(kk + 1) * 128], xe[:, b, k * 128:(k + 1) * 128], ident_b[:], ["xe", "ident_b"], [pk])
                            dst = xeT[:, k4 * 4:(k4 + 1) * 4, b * 128:(b + 1) * 128]
                            srcp = pt[:].rearrange("p (a q) -> p a q", a=4)
                            if k4 % 2:
                                ACT(dst, srcp, AF.Copy, [pk], ["xeT"])
                            else:
                                CP("vector", dst, srcp, [pk], ["xeT"])
                            pump(1)
                    w1k = [(w1b[s_].name, k4) for k4 in range(4)]
                    w3k = [(w3b[s_].name, k4) for k4 in range(4)]
                    w2k = [(w2b[s_].name, k) for k in range(4)]
                    for m in range(4):
                        b_ = m % 2
                        MMG(ph1[b_][:], [(w1b[s_][:, k, m * 128:(m + 1) * 128], xeT[:, k, :]) for k in range(16)],
                            w1k + ["xeT"], [("ph1", b_)])
                        MMG(ph3[b_][:], [(w3b[s_][:, k, m * 128:(m + 1) * 128], xeT[:, k, :]) for k in range(16)],
                            w3k + ["xeT"], [("ph3", b_)])
                        ACT(h1[:], ph1[b_][:], AF.Silu, [("ph1", b_)], ["h1"])
                        TT("vector", hT[:, m, :], h1[:], ph3[b_][:], ALU.mult, ["h1", ("ph3", b_)], ["hT"])
                        pump(2)
                    for b in range(2):
                        yb_ = yo[b]
                        for n_ in range(4):
                            pb = (b * 4 + n_) % 2
                            MMG(py[pb][:], [(hT[:, m, b * 128:(b + 1) * 128], w2b[s_][:, m, n_ * 512:(n_ + 1) * 512]) for m in range(4)],
                                w2k + ["hT"], [("py", pb)])
                            if n_ % 2:
                                ACT(yb_[:, n_ * 512:(n_ + 1) * 512], py[pb][:], AF.Copy, [("py", pb)], [yb_.name])
                            else:
                                CP("vector", yb_[:, n_ * 512:(n_ + 1) * 512], py[pb][:], [("py", pb)], [yb_.name])
                            pump(1)
                        DMA("scalar", ys_d[r0 + b * 128:r0 + (b + 1) * 128, :], yb_[:], "yst", [yb_.name], [("ys", e_, b)])
                    pump(99)

            with contextlib.ExitStack() as ph:
                ph.callback(S.barrier)
                g2 = sb(ph, "g2", [128, D])
                b2 = sb(ph, "b2", [128, D])
                ya = [sb(ph, f"ya{i}", [128, D]) for i in range(2)]
                yb = [sb(ph, f"yb{i}", [128, D]) for i in range(2)]
                xz = [sb(ph, f"xz{i}", [128, D]) for i in range(2)]
                oo = [sb(ph, f"oo{i}", [128, D]) for i in range(2)]
                st6 = sb(ph, "st6b", [128, 4, 6])
                mv = sb(ph, "mvb", [128, 2])
                sm = sb(ph, "smb", [128, 4])
                x1s_keys = [("x1s", t_) for t_ in range(16)]
                ys_keys = [("ys", e2, b2_) for e2 in range(NE) for b2_ in range(2)]
                DMA("sync", g2[:], ln2g_d, "k10", [], ["g2"])
                DMA("sync", b2[:], ln2b_d, "k11", [], ["b2"])
                for ti in range(16):
                    i_ = ti % 2
                    tk0 = ti * 128
                    MS("vector", ya[i_][:], 0.0, [f"ya{i_}"])
                    MS("vector", yb[i_][:], 0.0, [f"yb{i_}"])
                    for a_, dst, dk in ((0, ya[i_], f"ya{i_}"), (1, yb[i_], f"yb{i_}")):
                        GATH(dst[:], ys_d, slots_i[:, ti, a_:a_ + 1], f"gath{i_}{a_}", ys_keys + [("slots", ti)], [dk])
                    DMA(hwq(), xz[i_][:], x1s[tk0:tk0 + 128, :], f"xz{i_}", x1s_keys, [f"xz{i_}"])
                    ACT(xz[i_][:], xz[i_][:], AF.Copy, [f"xz{i_}"], [f"xz{i_}"], scale=ALPHA)
                    STT(xz[i_][:], ya[i_][:], wts[:, ti, 0:1], xz[i_][:], ALU.mult, ALU.add, [f"ya{i_}", "wts", f"xz{i_}"], [f"xz{i_}"])
                    STT(xz[i_][:], yb[i_][:], wts[:, ti, 1:2], xz[i_][:], ALU.mult, ALU.add, [f"yb{i_}", "wts", f"xz{i_}"], [f"xz{i_}"])
                    for nb_ in range(4):
                        BNS(st6[:, nb_, :], xz[i_][:, nb_ * 512:(nb_ + 1) * 512], [f"xz{i_}"], ["st6b"])
                    BNA(mv[:], st6[:].rearrange("p a b -> p (a b)"), ["st6b"], ["mvb"])
                    TS("vector", sm[:, 0:1], mv[:, 1:2], LN_EPS, None, ALU.add, None, ["mvb"], ["smb"])
                    ACT(sm[:, 0:1], sm[:, 0:1], AF.Sqrt, ["smb"], ["smb"])
                    REC(sm[:, 0:1], sm[:, 0:1], ["smb"], ["smb"])
                    STT(sm[:, 1:2], mv[:, 0:1], -1.0, sm[:, 0:1], ALU.mult, ALU.mult, ["mvb", "smb"], ["smb"])
                    ACT(oo[i_][:], xz[i_][:], AF.Identity, [f"xz{i_}", "smb"], [f"oo{i_}"], bias=sm[:, 1:2], scale=sm[:, 0:1])
                    TT("vector", oo[i_][:], oo[i_][:], g2[:], ALU.mult, [f"oo{i_}", "g2"], [f"oo{i_}"])
                    TT("vector", oo[i_][:], oo[i_][:], b2[:], ALU.add, [f"oo{i_}", "b2"], [f"oo{i_}"])
                    DMA("sync", out_d[tk0:tk0 + 128, :], oo[i_][:], "outst", [f"oo{i_}"], [], final=True)

        S.emit()
    return nc


def _host_consts():
    slopes = np.exp2(-8.0 * np.arange(1, NH + 1, dtype=np.float64) / NH)
    i = np.arange(128)[:, None]
    q = np.arange(128)[None, :]
    etab = np.zeros((128, 3, NH, 256), np.float32)
    for pi, d in enumerate(PATTERNS):
        for h in range(NH):
            dist0 = q + 128 - i
            dist1 = q - i
            etab[:, pi, h, 0:128] = np.where(dist0 <= 128, np.exp(-slopes[h] * d * np.minimum(dist0, 128)), 0.0)
            etab[:, pi, h, 128:256] = np.where(dist1 >= 0, np.exp(-slopes[h] * d * np.maximum(dist1, 0)), 0.0)
    ident = np.eye(128, dtype=np.float32)
    tri = (np.arange(128)[:, None] < np.arange(128)[None, :]).astype(np.float32)
    ebase = np.tile((np.arange(NE, dtype=np.float32) * CAP)[None, :], (128, 1))
    return etab, ident, tri, ebase


def _chunked(v):
    return np.ascontiguousarray(np.asarray(v, np.float32).reshape(8, 128).T)


def make_in_maps(x, w_in, conv_w, conv_b, lru_wa, lru_ba, lru_wx, lru_bx, lru_lambda, attn_norm_g, lru_norm_g,
                 w_out, ln1_g, ln1_b, router_grp_w, router_grp_b, router_exp_w, router_exp_b, w1, w3, w2,
                 ln2_g, ln2_b):
    f = lambda a: np.ascontiguousarray(np.asarray(a, np.float32))
    x = f(x)
    etab, ident, tri, ebase = _host_consts()
    convw = np.ascontiguousarray(f(conv_w)[0].reshape(4, 8, 128).transpose(2, 1, 0))

    def bd(wg):
        wg = f(wg)[0]
        o = np.zeros((128, 8, 128), np.float32)
        for n in range(16):
            c, hlf = n // 2, n % 2
            o[hlf * 64:(hlf + 1) * 64, c, hlf * 64:(hlf + 1) * 64] = wg[n]
        return o
    gcat = np.concatenate([_chunked(f(attn_norm_g)[0]), _chunked(f(lru_norm_g)[0])], axis=1)
    bc = lambda v: np.ascontiguousarray(np.broadcast_to(f(v).reshape(1, -1), (128, f(v).size)))
    wr = np.ascontiguousarray(np.concatenate([f(router_grp_w)[0], f(router_exp_w)[0]], axis=1))
    br = bc(np.concatenate([f(router_grp_b)[0], f(router_exp_b)[0]]))
    pk = lambda w: np.ascontiguousarray(f(w)[0].reshape(NE, 16, 128, DE).transpose(0, 2, 1, 3)).reshape(NE, 128, 16 * DE)
    wi_ = f(w_in)[0]
    wqkv = np.stack([np.concatenate([wi_[:, j * 1024 + pas * 256:j * 1024 + (pas + 1) * 256] for j in range(3)], axis=1) for pas in range(4)])
    wqkv = np.ascontiguousarray(wqkv.reshape(4, 16, 128, 768).transpose(0, 2, 1, 3)).reshape(4, 128, 16 * 768)
    shared = dict(
        w_in=wi_, w_out=f(w_out)[0], convw=convw, convb=_chunked(f(conv_b)[0]),
        lba=_chunked(f(lru_ba)[0].reshape(-1)), lbx=_chunked(f(lru_bx)[0].reshape(-1)), lam=_chunked(f(lru_lambda)[0]),
        wbda=bd(lru_wa), wbdx=bd(lru_wx), gcat=gcat,
        ln1g=bc(f(ln1_g)[0]), ln1b=bc(f(ln1_b)[0]), ln2g=bc(f(ln2_g)[0]), ln2b=bc(f(ln2_b)[0]),
        wr=wr, br=br, w1=pk(w1), w3=pk(w3), w2=f(w2)[0], wqkv=wqkv,
        etab=etab, ident=ident, tri=tri, ebase=ebase,
    )
    maps = []
    for c in range(8):
        b, h = c // 2, c % 2
        own = x[b, h * HALF:(h + 1) * HALF]
        prev = x[b, 0:HALF] if h == 1 else np.zeros((HALF, D), np.float32)
        xfull = np.concatenate([prev, own], axis=0)
        xT = np.ascontiguousarray(xfull.reshape(2 * HALF // LW, LW, 16, 128).transpose(0, 3, 2, 1)).reshape(2 * HALF // LW, 128, 16 * LW)
        m = dict(shared)
        m["xT"] = xT
        m["xtok"] = np.ascontiguousarray(own)
        m["flag"] = np.full((128, 1), float(h), np.float32)
        maps.append(m)
    return maps


def kernel(**inputs):
    nc = build_program()
    maps = make_in_maps(**inputs)
    res = run_bass_kernel_spmd(nc, maps, core_ids=list(range(8)))
    out = np.zeros((NB, SEQ, D), np.float32)
    for c in range(8):
        b, h = c // 2, c % 2
        out[b, h * HALF:(h + 1) * HALF] = np.asarray(res.results[c]["out"], np.float32)
    return out
```
